# Optimizing a Trainium2 kernel written in Bass

```python
import jax, jax.numpy as jnp
from jax import lax
import numpy as np

D_MODEL = 4096
BATCH = 8
SEQ = 2048
DEPTH = 2

RET_HEADS = 8
RET_DK = 256
RET_DV = 256
RET_CHUNK = 128
NSA_HEADS = 16
NSA_KV_HEADS = 4
NSA_DH = 128
CMP_BLOCK = 32
CMP_STRIDE = 16
CMP_HIDDEN = 128
SEL_BLOCK = 64
SEL_TOPK = 16
SEL_LOCAL = 2
SEL_QBLOCK = 16
WINDOW = 512
WIN_QBLOCK = 128
D_FF = 4 * D_MODEL
EPS = 1e-6
NEG_INF = -1e30
FORCED_SCORE = 1e4

RET_W = RET_HEADS * RET_DV
NSA_W = NSA_HEADS * NSA_DH
KV_W = NSA_KV_HEADS * NSA_DH
SPLITS = (RET_HEADS * RET_DK,
          RET_HEADS * RET_DK,
          RET_W,
          RET_W,
          NSA_W,
          6 * KV_W,
          3 * NSA_HEADS,
          D_MODEL,
          D_MODEL)
D_IN = sum(SPLITS)

kernel_name = "hybrid_retention_nsa_sqrelu_sandwich"


def rmsnorm(x, gain):
    xf = x.astype(jnp.float32)
    y = xf * lax.rsqrt(jnp.mean(xf * xf, axis=-1, keepdims=True) + EPS)
    return (y * gain.astype(jnp.float32)).astype(x.dtype)


def retention(q, k, v, g):
    B, T, H, DK = q.shape
    DV = v.shape[-1]
    C = RET_CHUNK
    N = T // C
    f32 = jnp.float32
    log_gamma = jnp.log1p(-jnp.exp2(-5.0 - jnp.arange(H, dtype=f32)))
    pos = jnp.arange(C, dtype=f32)
    rel = pos[:, None] - pos[None, :]
    d_intra = jnp.where(rel >= 0, jnp.exp(log_gamma[:, None, None] * jnp.maximum(rel, 0.0)), 0.0)
    d_query = jnp.exp(log_gamma[:, None] * (pos + 1.0))[..., None]
    d_key = jnp.exp(log_gamma[:, None] * (C - 1.0 - pos))[..., None]
    d_chunk = jnp.exp(log_gamma * C)[:, None, None]

    def to_chunks(a):
        return a.astype(f32).reshape(B, N, C, H, a.shape[-1]).transpose(1, 0, 3, 2, 4)

    qc, kc, vc = to_chunks(q), to_chunks(k * DK ** -0.5), to_chunks(v)

    def step(state, inp):
        qi, ki, vi = inp
        s = jnp.einsum('bhid,bhjd->bhij', qi, ki) * d_intra
        o = (jnp.einsum('bhij,bhje->bhie', s, vi)
             + jnp.einsum('bhid,bhde->bhie', qi, state) * d_query)
        state = state * d_chunk + jnp.einsum('bhjd,bhje->bhde', ki * d_key, vi)
        return state, o

    s0 = jnp.zeros((B, H, DK, DV), f32)
    _, o = lax.scan(step, s0, (qc, kc, vc))
    o = o.transpose(1, 0, 3, 2, 4).reshape(B, T, H, DV)
    mu = jnp.mean(o, axis=-1, keepdims=True)
    var = jnp.mean(jnp.square(o - mu), axis=-1, keepdims=True)
    o = (o - mu) * lax.rsqrt(var + EPS)
    y = jax.nn.silu(g.astype(f32)) * o
    return y.reshape(B, T, H * DV).astype(q.dtype)


def compress_blocks(x, pos_emb, w1, w2):
    B, T, KVH, DH = x.shape
    n_cmp = (T - CMP_BLOCK) // CMP_STRIDE + 1
    idx = CMP_STRIDE * jnp.arange(n_cmp)[:, None] + jnp.arange(CMP_BLOCK)[None, :]
    blocks = x[:, idx] + pos_emb[:, None, :]
    flat = blocks.transpose(0, 1, 3, 2, 4).reshape(B, n_cmp, KVH, CMP_BLOCK * DH)
    return jax.nn.silu(flat @ w1) @ w2


def nsa(q, k_cmp, v_cmp, k_sel, v_sel, k_win, v_win, gate_logits,
        pos_k, w1_k, w2_k, pos_v, w1_v, w2_v):
    B, T, H, DH = q.shape
    KVH = k_cmp.shape[2]
    G = H // KVH
    f32 = jnp.float32
    scale = DH ** -0.5
    slopes = jnp.exp2(-8.0 * (jnp.arange(H, dtype=f32) + 1.0) / H).reshape(KVH, G)
    qg = q.reshape(B, T, KVH, G, DH)
    t_pos = jnp.arange(T)

    kc = compress_blocks(k_cmp, pos_k, w1_k, w2_k)
    vc = compress_blocks(v_cmp, pos_v, w1_v, w2_v)
    n_cmp = kc.shape[1]
    cmp_start = CMP_STRIDE * jnp.arange(n_cmp)
    dist_c = t_pos[:, None] - (cmp_start + CMP_BLOCK - 1)[None, :]
    valid_c = dist_c >= 0
    s_c = (jnp.einsum('btgrd,bngd->bgrtn', qg, kc).astype(f32) * scale
           - slopes[:, :, None, None] * dist_c)
    p_c = jnp.where(valid_c, jax.nn.softmax(jnp.where(valid_c, s_c, NEG_INF), axis=-1), 0.0)
    o_cmp = jnp.einsum('bgrtn,bngd->btgrd', p_c, vc.astype(f32))

    n_sel = T // SEL_BLOCK
    k_top = min(SEL_TOPK, n_sel)
    sel_start = SEL_BLOCK * jnp.arange(n_sel)
    overlap = jnp.clip(jnp.minimum(cmp_start[:, None] + CMP_BLOCK, sel_start[None, :] + SEL_BLOCK)
                       - jnp.maximum(cmp_start[:, None], sel_start[None, :]), 0).astype(f32) / CMP_BLOCK
    imp = jnp.einsum('bgrtn,nj->bgtj', p_c, overlap)
    blk = jnp.arange(n_sel)[None, :]
    cur = (t_pos // SEL_BLOCK)[:, None]
    future = sel_start[None, :] > t_pos[:, None]
    forced = (blk == 0) | ((cur - blk >= 0) & (cur - blk < SEL_LOCAL))
    imp = jnp.where(forced, FORCED_SCORE, jnp.where(future, -1.0, imp))
    _, sel_idx = lax.top_k(imp, k_top)

    ksb = k_sel.reshape(B, n_sel, SEL_BLOCK, KVH, DH).transpose(0, 3, 1, 2, 4)
    vsb = v_sel.reshape(B, n_sel, SEL_BLOCK, KVH, DH).transpose(0, 3, 1, 2, 4)
    nq = T // SEL_QBLOCK
    q_chunks = qg.reshape(B, nq, SEL_QBLOCK, KVH, G, DH).transpose(1, 0, 2, 3, 4, 5)
    i_chunks = sel_idx.reshape(B, KVH, nq, SEL_QBLOCK, k_top).transpose(2, 0, 1, 3, 4)
    t_chunks = t_pos.reshape(nq, SEL_QBLOCK)
    bi = jnp.arange(B)[:, None, None, None]
    gi = jnp.arange(KVH)[None, :, None, None]
    offs = jnp.arange(SEL_BLOCK)

    def sel_step(args):
        qc, ic, tc = args
        kg = ksb[bi, gi, ic]
        vg = vsb[bi, gi, ic]
        dist = tc[None, None, :, None, None] - (ic[..., None] * SEL_BLOCK + offs)
        valid = (dist >= 0)[:, :, None]
        s = (jnp.einsum('bqgrd,bgqksd->bgrqks', qc, kg).astype(f32) * scale
             - slopes[None, :, :, None, None, None] * dist[:, :, None])
        p = jax.nn.softmax(jnp.where(valid, s, NEG_INF), axis=(-2, -1))
        return jnp.einsum('bgrqks,bgqksd->bqgrd', p, vg.astype(f32))

    o_sel = lax.map(sel_step, (q_chunks, i_chunks, t_chunks))
    o_sel = o_sel.transpose(1, 0, 2, 3, 4, 5).reshape(B, T, KVH, G, DH)

    nw = T // WIN_QBLOCK
    span = WINDOW + WIN_QBLOCK
    kwp = jnp.pad(k_win, ((0, 0), (WINDOW, 0), (0, 0), (0, 0)))
    vwp = jnp.pad(v_win, ((0, 0), (WINDOW, 0), (0, 0), (0, 0)))
    qw = qg.reshape(B, nw, WIN_QBLOCK, KVH, G, DH).transpose(1, 0, 2, 3, 4, 5)
    rows = jnp.arange(WIN_QBLOCK)
    cols = jnp.arange(span)

    def win_step(args):
        qb, i = args
        start = i * WIN_QBLOCK
        kb = lax.dynamic_slice_in_dim(kwp, start, span, axis=1)
        vb = lax.dynamic_slice_in_dim(vwp, start, span, axis=1)
        tq = start + rows
        tk = start - WINDOW + cols
        dist = tq[:, None] - tk[None, :]
        valid = (dist >= 0) & (dist < WINDOW) & (tk[None, :] >= 0)
        s = (jnp.einsum('bqgrd,bkgd->bgrqk', qb, kb).astype(f32) * scale
             - slopes[:, :, None, None] * dist)
        p = jax.nn.softmax(jnp.where(valid, s, NEG_INF), axis=-1)
        return jnp.einsum('bgrqk,bkgd->bqgrd', p, vb.astype(f32))

    o_win = lax.map(win_step, (qw, jnp.arange(nw)))
    o_win = o_win.transpose(1, 0, 2, 3, 4, 5).reshape(B, T, KVH, G, DH)

    gates = jax.nn.sigmoid(gate_logits.astype(f32)).reshape(B, T, KVH, G, 3, 1)
    o = gates[..., 0, :] * o_cmp + gates[..., 1, :] * o_sel + gates[..., 2, :] * o_win
    return o.reshape(B, T, H * DH).astype(q.dtype)


def setup_inputs(seed: int = 0) -> dict:
    key = jax.random.key(seed)
    ks = jax.random.split(key, 17)
    f32 = jnp.float32

    def nrm(k, shape, fan_in):
        return jax.random.normal(k, shape, f32) * (fan_in ** -0.5)

    def gain(k):
        return 1.0 + 0.05 * jax.random.normal(k, (DEPTH, D_MODEL), f32)

    return {
        "x": jax.random.normal(ks[0], (BATCH, SEQ, D_MODEL), f32),
        "mix_norm_pre": gain(ks[1]),
        "w_in": nrm(ks[2], (DEPTH, D_MODEL, D_IN), D_MODEL),
        "cmp_pos_k": 0.1 * jax.random.normal(ks[3], (DEPTH, CMP_BLOCK, NSA_DH), f32),
        "cmp_w1_k": nrm(ks[4], (DEPTH, CMP_BLOCK * NSA_DH, CMP_HIDDEN), CMP_BLOCK * NSA_DH),
        "cmp_w2_k": nrm(ks[5], (DEPTH, CMP_HIDDEN, NSA_DH), CMP_HIDDEN),
        "cmp_pos_v": 0.1 * jax.random.normal(ks[6], (DEPTH, CMP_BLOCK, NSA_DH), f32),
        "cmp_w1_v": nrm(ks[7], (DEPTH, CMP_BLOCK * NSA_DH, CMP_HIDDEN), CMP_BLOCK * NSA_DH),
        "cmp_w2_v": nrm(ks[8], (DEPTH, CMP_HIDDEN, NSA_DH), CMP_HIDDEN),
        "w_ret_up": nrm(ks[9], (DEPTH, RET_W, D_MODEL), RET_W),
        "w_nsa_up": nrm(ks[10], (DEPTH, NSA_W, D_MODEL), NSA_W),
        "w_out": nrm(ks[11], (DEPTH, D_MODEL, D_MODEL), D_MODEL),
        "mix_norm_post": gain(ks[12]),
        "mlp_norm_pre": gain(ks[13]),
        "w_mlp_in": nrm(ks[14], (DEPTH, D_MODEL, D_FF), D_MODEL),
        "w_mlp_out": nrm(ks[15], (DEPTH, D_FF, D_MODEL), D_FF),
        "mlp_norm_post": gain(ks[16]),
    }


def reference(x, mix_norm_pre, w_in, cmp_pos_k, cmp_w1_k, cmp_w2_k, cmp_pos_v, cmp_w1_v,
              cmp_w2_v, w_ret_up, w_nsa_up, w_out, mix_norm_post, mlp_norm_pre, w_mlp_in,
              w_mlp_out, mlp_norm_post):
    B, T, _ = x.shape
    cuts = np.cumsum(np.array(SPLITS))[:-1].tolist()
    for l in range(DEPTH):
        h = rmsnorm(x, mix_norm_pre[l])
        q_r, k_r, v_r, g_r, q_n, kv_n, gate_n, gate_ret, gate_nsa = jnp.split(h @ w_in[l], cuts, axis=-1)
        k_c, v_c, k_s, v_s, k_w, v_w = [a.reshape(B, T, NSA_KV_HEADS, NSA_DH)
                                        for a in jnp.split(kv_n, 6, axis=-1)]
        y_ret = retention(q_r.reshape(B, T, RET_HEADS, RET_DK),
                          k_r.reshape(B, T, RET_HEADS, RET_DK),
                          v_r.reshape(B, T, RET_HEADS, RET_DV),
                          g_r.reshape(B, T, RET_HEADS, RET_DV))
        y_nsa = nsa(q_n.reshape(B, T, NSA_HEADS, NSA_DH), k_c, v_c, k_s, v_s, k_w, v_w, gate_n,
                    cmp_pos_k[l], cmp_w1_k[l], cmp_w2_k[l], cmp_pos_v[l], cmp_w1_v[l], cmp_w2_v[l])
        merged = (jax.nn.sigmoid(gate_ret) * (y_ret @ w_ret_up[l])
                  + jax.nn.sigmoid(gate_nsa) * (y_nsa @ w_nsa_up[l]))
        x = x + rmsnorm(merged @ w_out[l], mix_norm_post[l])
        h = rmsnorm(x, mlp_norm_pre[l])
        u = jnp.square(jax.nn.relu(h @ w_mlp_in[l]))
        x = x + rmsnorm(u @ w_mlp_out[l], mlp_norm_post[l])
    return x
```

```python
import numpy as np
import concourse.bass as bass
import concourse.mybir as mybir

F32 = mybir.dt.float32
BF16 = mybir.dt.bfloat16
I32 = mybir.dt.int32
ACT = mybir.ActivationFunctionType
ALU = mybir.AluOpType
AX = mybir.AxisListType


class TL:
    __slots__ = ("name", "step", "n", "marks", "handle", "cmap", "phys", "base")

    def __init__(self, name, step):
        self.name, self.step, self.n, self.marks, self.handle, self.cmap = name, step, 0, set(), None, None
        self.phys, self.base = None, 0


class PhysSem:
    __slots__ = ("name", "total", "handle")

    def __init__(self, name):
        self.name, self.total, self.handle = name, 0, None


class Buf:
    __slots__ = ("name", "last_w", "readers", "tl")

    def __init__(self, name):
        self.name, self.last_w, self.readers, self.tl = name, None, {}, None


class V:
    __slots__ = ("ap", "buf")

    def __init__(self, ap, buf):
        self.ap, self.buf = ap, buf

    def __getitem__(self, idx):
        return V(self.ap[idx], self.buf)

    def re(self, pat, **kw):
        return V(self.ap.rearrange(pat, **kw), self.buf)


class Prog:
    ENGS = ("pe", "act", "dve", "pool", "sp")

    def __init__(self, nc):
        self.nc = nc
        self.tl = {e: TL(e, 1) for e in self.ENGS}
        self.seen = {e: {} for e in self.ENGS}
        self.ops = {e: [] for e in self.ENGS}
        self.dma_tls = []
        self.stack = []
        self.nbuf = 0
        self.free_phys = []
        self.all_phys = []
        for e in self.ENGS:
            ph = PhysSem("eng_" + e)
            self.all_phys.append(ph)
            self.tl[e].phys = ph

    def sb(self, name, shape, dt):
        g = self.nc.sbuf_tensor(name + "_%d" % self.nbuf, list(shape), dt)
        self.nbuf += 1
        t = g.__enter__()
        self.stack.append(g)
        return V(t[:] if not hasattr(t, "ap") else t.ap(), Buf(name))

    def ps(self, name, shape, dt=F32):
        g = self.nc.psum_tensor(name + "_%d" % self.nbuf, list(shape), dt)
        self.nbuf += 1
        t = g.__enter__()
        self.stack.append(g)
        return V(t[:] if not hasattr(t, "ap") else t.ap(), Buf(name))

    def mark(self):
        return (len(self.stack), len(self.dma_tls))

    def release(self, mark):
        ns, nt = mark
        while len(self.stack) > ns:
            self.stack.pop().__exit__(None, None, None)
        for t in self.dma_tls[nt:]:
            if t.phys is not None:
                t.phys.total += len(t.marks) * t.step
                self.free_phys.append(t.phys)
                t.phys = None
                t.cmap = "retired"
        self.retired = getattr(self, "retired", [])
        self.retired.extend(self.dma_tls[nt:])
        del self.dma_tls[nt:]

    def dma_tl(self, name):
        t = TL(name, 16)
        if self.free_phys:
            t.phys = self.free_phys.pop()
        else:
            t.phys = PhysSem("dsem%d" % len(self.all_phys))
            self.all_phys.append(t.phys)
        t.base = t.phys.total
        t.handle = t.phys
        self.dma_tls.append(t)
        return t

    def op(self, eng, fn, reads=(), writes=(), tl=None, extra_deps=()):
        own = self.tl[eng]
        tline = tl or own
        seen = self.seen[eng]
        deps = list(extra_deps)
        for b in reads:
            if b.last_w is not None:
                deps.append(b.last_w)
        for b in writes:
            if b.last_w is not None and b.last_w[0] is not own:
                deps.append(b.last_w)
            for s, i in b.readers.items():
                if s is not own:
                    deps.append((s, i))
        waits = {}
        for s, i in deps:
            if s is own:
                if eng == "pe" or eng == "sp":
                    continue
            if seen.get(s, 0) >= i:
                continue
            if waits.get(s, 0) < i:
                waits[s] = i
        for s, i in waits.items():
            seen[s] = i
            s.marks.add(i)
        tline.n += 1
        idx = tline.n
        self.ops[eng].append((fn, list(waits.items()), (tline, idx)))
        for b in reads:
            if b.readers.get(tline, 0) < idx:
                b.readers[tline] = idx
        for b in writes:
            b.last_w = (tline, idx)
            b.readers = {}
        return (tline, idx)

    def barrier(self):
        toks = []
        for e in self.ENGS:
            deps = []
            if self.tl[e].n > 0:
                deps.append((self.tl[e], self.tl[e].n))
            if e == "sp":
                for t in self.dma_tls:
                    if t.n > 0:
                        deps.append((t, t.n))
            own = self.tl[e]
            waits = {}
            for s, i in deps:
                if self.seen[e].get(s, 0) >= i and s is not own:
                    continue
                waits[s] = i
                s.marks.add(i)
                self.seen[e][s] = i
            own.n += 1
            idx = own.n
            self.ops[e].append((lambda en: en.nop(), list(waits.items()), (own, idx)))
            toks.append((own, idx))
        for e in self.ENGS:
            waits = {}
            for s, i in toks:
                if s is self.tl[e]:
                    continue
                waits[s] = i
                s.marks.add(i)
                self.seen[e][s] = i
            for t in self.dma_tls:
                self.seen[e][t] = t.n
            own = self.tl[e]
            own.n += 1
            self.ops[e].append((lambda en: en.nop(), list(waits.items()), (own, own.n)))

    def emit(self):
        nc = self.nc
        guards = []
        for ph in self.all_phys:
            g = nc.semaphore(ph.name)
            ph.handle = g.__enter__()
            guards.append(g)
        tls = list(self.tl.values()) + self.dma_tls + getattr(self, "retired", [])
        for t in tls:
            ph = t.handle if isinstance(t.handle, PhysSem) else t.phys
            t.handle = ph.handle
            ms = sorted(t.marks)
            t.cmap = {m: t.base + (k + 1) * t.step for k, m in enumerate(ms)}
        print("semaphores used:", len(self.all_phys), "ops:", {e: len(v) for e, v in self.ops.items()})
        engmap = {"pe": "tensor", "act": "scalar", "dve": "vector", "pool": "gpsimd", "sp": "sync"}
        with nc.Block() as block:
            for e in self.ENGS:
                ops = self.ops[e]

                def body(en, ops=ops):
                    for fn, waits, (tline, idx) in ops:
                        for s, i in waits:
                            en.wait_ge(s.handle, s.cmap[i])
                        ins = fn(en)
                        if idx in tline.marks:
                            ins.then_inc(tline.handle, tline.step)
                getattr(block, engmap[e])(body)
        for g in reversed(guards):
            g.__exit__(None, None, None)


    @staticmethod
    def _a(x):
        return x.ap if isinstance(x, V) else x

    @staticmethod
    def _b(*xs):
        return [x.buf for x in xs if isinstance(x, V)]

    def dma(self, q, out, in_, side):
        b = side.buf
        if b.tl is None:
            b.tl = self.dma_tl("d_" + b.name)
        o, i = out.ap, in_.ap
        return self.op(q, lambda en: en.dma_start(out=o, in_=i), reads=[in_.buf], writes=[out.buf], tl=b.tl)

    def mm(self, out, lhsT, rhs, start=True, stop=True):
        o, l, r = out.ap, lhsT.ap, rhs.ap
        return self.op("pe", lambda en: en.matmul(o, l, r, start=start, stop=stop),
                       reads=[lhsT.buf, rhs.buf], writes=[out.buf])

    def tr(self, out, in_, ident):
        o, i, d = out.ap, in_.ap, ident.ap
        return self.op("pe", lambda en: en.transpose(o, i, d), reads=[in_.buf, ident.buf], writes=[out.buf])

    def act(self, out, in_, func, bias=0.0, scale=1.0, accum=None):
        o, i, b, s = out.ap, in_.ap, self._a(bias), self._a(scale)
        ac = accum.ap if accum is not None else None
        w = [out.buf] + ([accum.buf] if accum is not None else [])
        if ac is None:
            f = lambda en: en.activation(out=o, in_=i, func=func, bias=b, scale=s)
        else:
            f = lambda en: en.activation(out=o, in_=i, func=func, bias=b, scale=s, accum_out=ac)
        return self.op("act", f, reads=[in_.buf] + self._b(bias, scale), writes=w)

    def ts(self, eng, out, in0, s1, s2, op0, op1=None):
        o, i, a1, a2 = out.ap, in0.ap, self._a(s1), self._a(s2)
        if op1 is None:
            f = lambda en: en.tensor_scalar(o, i, a1, None, op0)
        else:
            f = lambda en: en.tensor_scalar(o, i, a1, a2, op0, op1)
        return self.op(eng, f, reads=[in0.buf] + self._b(s1, s2), writes=[out.buf])

    def tt(self, eng, out, in0, in1, op):
        o, a, b = out.ap, in0.ap, in1.ap
        return self.op(eng, lambda en: en.tensor_tensor(o, a, b, op), reads=[in0.buf, in1.buf], writes=[out.buf])

    def stt(self, eng, out, in0, scalar, in1, op0, op1):
        o, a, s, b = out.ap, in0.ap, self._a(scalar), in1.ap
        return self.op(eng, lambda en: en.scalar_tensor_tensor(o, a, s, b, op0, op1),
                       reads=[in0.buf, in1.buf] + self._b(scalar), writes=[out.buf])

    def red(self, out, in_, op, axis=None):
        o, i = out.ap, in_.ap
        ax = axis or AX.X
        return self.op("dve", lambda en: en.tensor_reduce(o, i, ax, op), reads=[in_.buf], writes=[out.buf])

    def copy(self, eng, out, in_):
        o, i = out.ap, in_.ap
        if eng == "act":
            f = lambda en: en.copy(o, i)
        else:
            f = lambda en: en.tensor_copy(o, i)
        return self.op(eng, f, reads=[in_.buf], writes=[out.buf])

    def memset(self, eng, out, val):
        o = out.ap
        return self.op(eng, lambda en: en.memset(o, val), writes=[out.buf])

    def recip(self, out, in_):
        o, i = out.ap, in_.ap
        return self.op("dve", lambda en: en.reciprocal(o, i), reads=[in_.buf], writes=[out.buf])


import math

T = 2048
D = 4096
DIN = 21552
DFF = 16384
EPS = 1e-6
NL = 2


class Rot:
    def __init__(self, items):
        self.items, self.i = items, 0

    def next(self):
        x = self.items[self.i % len(self.items)]
        self.i += 1
        return x


class Ctx:
    def __init__(self, nc, dbg=()):
        self.nc = nc
        self.P = Prog(nc)
        self.dbg = set(dbg)
        self.dram = {}
        self.wbuf = Buf("weights")
        P = self.P
        self.banks = [P.ps("bank%d" % i, [128, 512], F32) for i in range(8)]
        self.bank_rot = Rot(self.banks)
        self.ident = P.sb("ident", [128, 128], BF16)
        self.identf = P.sb("identf", [128, 128], F32)
        for idt in (self.ident, self.identf):
            P.memset("pool", idt, 0.0)
            o = idt.ap
            P.op("pool", lambda en, o=o: en.affine_select(o, o, [[-1, 128]], ALU.not_equal, 1.0, base=0, channel_multiplier=1),
                 reads=[idt.buf], writes=[idt.buf])
        self.altc = 0

    def next_ps(self):
        return self.bank_rot.next()

    def alt(self):
        self.altc += 1
        return "act" if self.altc % 2 else "dve"

    def inp(self, name, shape, dt=F32):
        t = self.nc.dram_tensor(name, list(shape), dt, kind="ExternalInput")
        v = V(t.ap(), self.wbuf)
        self.dram[name] = v
        return v

    def scratch(self, name, shape, dt):
        kind = "ExternalOutput" if name in self.dbg else "Internal"
        t = self.nc.dram_tensor(name, list(shape), dt, kind=kind)
        v = V(t.ap(), Buf(name))
        self.dram[name] = v
        return v


def psb(v):
    return V(v.ap.bitcast(BF16), v.buf)


def phase_nr_a(C, z, x_src, x_dst, g_post, g_pre, HT):
    P = C.P
    m = P.mark()
    need_nr = z is not None
    need_a = HT is not None
    xt = [P.sb("xt%d" % i, [128, D], F32) for i in range(3)]
    junk = P.sb("junk", [128, D], BF16)
    junk2 = P.sb("junk2", [128, D], BF16)
    if need_nr:
        zt = [P.sb("zt%d" % i, [128, D], F32) for i in range(3)]
        gpost = P.sb("gpost", [128, D], F32)
        P.dma("sp", gpost, V(g_post.ap.partition_broadcast(128), g_post.buf), gpost)
        ssz = [P.sb("ssz%d" % i, [128, 1], F32) for i in range(2)]
        rz = [P.sb("rz%d" % i, [128, 1], F32) for i in range(2)]
    if need_a:
        gpre = P.sb("gpre", [128, D], F32)
        P.dma("sp", gpre, V(g_pre.ap.partition_broadcast(128), g_pre.buf), gpre)
        hb = [P.sb("hb%d" % i, [128, D], BF16) for i in range(2)]
        hs = [P.sb("hs%d" % i, [128, 32, 256], BF16) for i in range(2)]
        ssx = [P.sb("ssx%d" % i, [128, 1], F32) for i in range(2)]
        rx = [P.sb("rx%d" % i, [128, 1], F32) for i in range(2)]
        HTv = HT.re("(kc p) t -> p kc t", p=128)
    def stage1(i):
        j = i % 2
        j3 = i % 3
        rows = slice(i * 128, (i + 1) * 128)
        P.dma("sp", xt[j3], x_src[rows, :], xt[j3])
        if need_nr:
            P.dma("sp", zt[j3], z[rows, :], zt[j3])
            P.act(junk, zt[j3], ACT.Square, accum=ssz[j])
            P.act(ssz[j], ssz[j], ACT.Sqrt, bias=EPS, scale=1.0 / D)
            P.recip(rz[j], ssz[j])
            P.stt("dve", zt[j3], zt[j3], rz[j][:, 0:1], gpost, ALU.mult, ALU.mult)
            P.tt("pool", xt[j3], xt[j3], zt[j3], ALU.add)
            P.dma("pool", x_dst[rows, :], xt[j3], xt[j3])

    def stage2(i):
        j = i % 2
        j3 = i % 3
        if need_a:
            P.act(junk2, xt[j3], ACT.Square, accum=ssx[j])
            P.act(ssx[j], ssx[j], ACT.Sqrt, bias=EPS, scale=1.0 / D)
            P.recip(rx[j], ssx[j])
            P.stt("dve", hb[j], xt[j3], rx[j][:, 0:1], gpre, ALU.mult, ALU.mult)
            hsl = hs[(i // 2) % 2]
            half = (i % 2) * 128
            for b in range(4):
                bank = C.next_ps()
                pb = psb(bank).re("p (a t) -> p a t", a=8)
                for a in range(8):
                    kc = b * 8 + a
                    P.tr(pb[:, a, :], hb[j][:, kc * 128:(kc + 1) * 128], C.ident)
                P.copy(C.alt(), hsl[:, b * 8:(b + 1) * 8, half:half + 128], pb)
            if i % 2 == 1:
                t0 = (i - 1) * 128
                for h2 in range(2):
                    P.dma("act", HTv[:, h2 * 16:(h2 + 1) * 16, t0:t0 + 256], hsl[:, h2 * 16:(h2 + 1) * 16, :], hsl)

    nt = T // 128
    for k in range(nt + 1):
        if k < nt:
            stage1(k)
        if k >= 1:
            stage2(k - 1)
    P.barrier()
    P.release(m)


class Epi:
    def __init__(self, C, st32, st16):
        self.C, self.st32, self.st16 = C, st32, st16

    def make(self, kind, dst, seg0, orient):
        C, P = self.C, self.C.P

        def epi(ps, w, a0, b0):
            if orient == "F":
                src = ps[0:w, :]
                sl = lambda st: st[0:w, :]
                d = dst[a0 - seg0:a0 - seg0 + w, b0:b0 + 512]
            else:
                src = ps[:, 0:w]
                sl = lambda st: st[:, 0:w]
                d = dst[a0:a0 + 128, b0 - seg0:b0 - seg0 + w]
            if kind == "bf16":
                st = self.st16.next()
                P.copy(C.alt(), sl(st), src)
            elif kind == "f32":
                st = self.st32.next()
                P.copy(C.alt(), sl(st), src)
            elif kind == "sig32":
                st = self.st32.next()
                P.act(sl(st), src, ACT.Sigmoid)
            elif kind == "silu32":
                st = self.st32.next()
                P.act(sl(st), src, ACT.Silu)
            elif kind == "relu2":
                tmp = self.st32.next()
                st = self.st16.next()
                P.act(sl(tmp), src, ACT.Relu)
                P.tt("dve", sl(st), sl(tmp), sl(tmp), ALU.mult)
            else:
                raise ValueError(kind)
            P.dma("sp", d, sl(st), st)
        return epi


def gemm(C, src, K, W, blocks, TT, KB=32, nwb=2, bg=None):
    P = C.P
    m = P.mark()
    bg = list(bg or [])
    bgside = V(None, Buf("bgcast"))

    def bg_issue(n=1):
        for _ in range(n):
            if bg:
                o, i = bg.pop(0)
                P.dma("pool", o, i, bgside)
    KC = K // 128
    KB = min(KC, KB)
    NKB = KC // KB
    A = P.sb("A", [128, KC, TT], BF16)
    wb = Rot([P.sb("wb%d" % i, [128, KB, 512], BF16) for i in range(nwb)])
    st32 = Rot([P.sb("st32_%d" % i, [128, 512], F32) for i in range(3)])
    st16 = Rot([P.sb("st16_%d" % i, [128, 512], BF16) for i in range(3)])
    E = Epi(C, st32, st16)
    Wv = W.re("(kc p) n -> p kc n", p=128)
    srcv = src.re("(kc p) t -> p kc t", p=128)
    for t0 in range(0, T, TT):
        for k0 in range(0, KC, 8):
            P.dma("sp", A[:, k0:k0 + 8, :], srcv[:, k0:k0 + 8, t0:t0 + TT], A)
        for (c0, ncols, orient, kind, dst, seg0) in blocks:
            epi = E.make(kind, dst, seg0, orient)
            if orient == "F":
                assert NKB == 1
                w = wb.next()
                P.dma("pool", w[:, :, 0:ncols], Wv[:, :, c0:c0 + ncols], w)
                bg_issue(1)
                for fc in range(0, ncols, 128):
                    fw = min(128, ncols - fc)
                    for tg in range(0, TT, 512):
                        ps = C.next_ps()
                        for kc in range(KC):
                            P.mm(ps[0:fw, :], w[:, kc, fc:fc + fw], A[:, kc, tg:tg + 512], start=(kc == 0), stop=(kc == KC - 1))
                        epi(ps, fw, c0 + fc, t0 + tg)
            else:
                ntt = TT // 128
                if NKB == 1:
                    w = wb.next()
                    P.dma("pool", w[:, :, 0:ncols], Wv[:, :, c0:c0 + ncols], w)
                    for tt in range(ntt):
                        ps = C.next_ps()
                        for kc in range(KC):
                            P.mm(ps[:, 0:ncols], A[:, kc, tt * 128:(tt + 1) * 128], w[:, kc, 0:ncols], start=(kc == 0), stop=(kc == KC - 1))
                        epi(ps, ncols, t0 + tt * 128, c0)
                else:
                    pss = [C.next_ps() for _ in range(ntt)]
                    for kb in range(NKB):
                        w = wb.next()
                        P.dma("pool", w[:, :, 0:ncols], Wv[:, kb * KB:(kb + 1) * KB, c0:c0 + ncols], w)
                        for tt in range(ntt):
                            for kc in range(KB):
                                P.mm(pss[tt][:, 0:ncols], A[:, kb * KB + kc, tt * 128:(tt + 1) * 128], w[:, kc, 0:ncols],
                                     start=(kb == 0 and kc == 0), stop=(kb == NKB - 1 and kc == KB - 1))
                    for tt in range(ntt):
                        epi(pss[tt], ncols, t0 + tt * 128, c0)
    bg_issue(len(bg))
    P.barrier()
    P.release(m)


def seg_blocks(c0, n, orient, kind, dst, bw=512):
    out = []
    for c in range(c0, c0 + n, bw):
        out.append((c, min(bw, c0 + n - c), orient, kind, dst, c0))
    return out


def phase_up(C, YRT, YNT, Wr, Wn, GRETT, GNSAT, MT):
    P = C.P
    m = P.mark()
    TT = T
    BW = 256
    Ar = P.sb("Ar", [128, 16, TT], BF16)
    An = P.sb("An", [128, 16, TT], BF16)
    wr = Rot([P.sb("wr%d" % i, [128, 16, BW], BF16) for i in range(2)])
    wn = Rot([P.sb("wn%d" % i, [128, 16, BW], BF16) for i in range(2)])
    grt = Rot([P.sb("grt%d" % i, [128, 512], F32) for i in range(4)])
    gnt = Rot([P.sb("gnt%d" % i, [128, 512], F32) for i in range(4)])
    tmp = Rot([P.sb("tmp%d" % i, [128, 512], F32) for i in range(3)])
    st16 = Rot([P.sb("st16u_%d" % i, [128, 512], BF16) for i in range(3)])
    Wrv = Wr.re("(kc p) n -> p kc n", p=128)
    Wnv = Wn.re("(kc p) n -> p kc n", p=128)
    yrv = YRT.re("(kc p) t -> p kc t", p=128)
    ynv = YNT.re("(kc p) t -> p kc t", p=128)
    for k0 in range(0, 16, 4):
        P.dma("sp", Ar[:, k0:k0 + 4, :], yrv[:, k0:k0 + 4, :], Ar)
        P.dma("sp", An[:, k0:k0 + 4, :], ynv[:, k0:k0 + 4, :], An)
    for nb in range(D // BW):
        a, b = wr.next(), wn.next()
        P.dma("pool", a, Wrv[:, :, nb * BW:(nb + 1) * BW], a)
        P.dma("pool", b, Wnv[:, :, nb * BW:(nb + 1) * BW], b)
        for fc in range(BW // 128):
            f0 = nb * BW + fc * 128
            for tg in range(0, TT, 512):
                psr, psn = C.next_ps(), C.next_ps()
                for kc in range(16):
                    P.mm(psr, a[:, kc, fc * 128:(fc + 1) * 128], Ar[:, kc, tg:tg + 512], start=(kc == 0), stop=(kc == 15))
                for kc in range(16):
                    P.mm(psn, b[:, kc, fc * 128:(fc + 1) * 128], An[:, kc, tg:tg + 512], start=(kc == 0), stop=(kc == 15))
                g1, g2, t1, st = grt.next(), gnt.next(), tmp.next(), st16.next()
                tok = slice(tg, tg + 512)
                P.dma("act", g1, GRETT[f0:f0 + 128, tok], g1)
                P.dma("act", g2, GNSAT[f0:f0 + 128, tok], g2)
                P.tt("dve", g1, psr, g1, ALU.mult)
                P.tt("dve", t1, psn, g2, ALU.mult)
                P.tt("pool", st, t1, g1, ALU.add)
                P.dma("sp", MT[f0:f0 + 128, tok], st, st)
    P.barrier()
    P.release(m)


def mkap(v, dims):
    a = v.ap
    return V(bass.AP(a.tensor, a.offset, [list(a.ap[0])] + [list(d) for d in dims]), v.buf)


def phase_ret(C, QRT, KRT, KR, VR, GR, YRT):
    P = C.P
    m = P.mark()
    H = 8
    lgs = [math.log1p(-2.0 ** (-5.0 - h)) for h in range(H)]
    LN16 = math.log(1.0 / 16.0)
    val = P.sb("val", [128, 128], F32)
    o = val.ap
    P.op("pool", lambda en: en.iota(o, [[1, 128]], base=0, channel_multiplier=-1, allow_small_or_imprecise_dtypes=True), writes=[val.buf])
    P.ts("pool", val, val, 0.0, None, ALU.max)
    pidx = P.sb("pidx", [128, 1], F32)
    o2 = pidx.ap
    P.op("pool", lambda en: en.iota(o2, [[0, 1]], base=0, channel_multiplier=1, allow_small_or_imprecise_dtypes=True), writes=[pidx.buf])
    DT = P.sb("DT", [128, H, 128], F32)
    dq = P.sb("dq", [128, H], F32)
    dk = P.sb("dk", [128, H], F32)
    for h in range(H):
        P.act(DT[:, h, :], val, ACT.Exp, bias=LN16, scale=lgs[h])
        oo = DT[:, h, :].ap
        P.op("pool", lambda en, oo=oo: en.affine_select(oo, oo, [[1, 128]], ALU.is_ge, 0.0, base=0, channel_multiplier=-1),
             reads=[DT.buf], writes=[DT.buf])
        P.act(dq[:, h:h + 1], pidx, ACT.Exp, bias=lgs[h], scale=lgs[h])
        P.act(dk[:, h:h + 1], pidx, ACT.Exp, bias=127.0 * lgs[h] + LN16, scale=-lgs[h])
    qT = [P.sb("qT%d" % i, [128, 2, T], BF16) for i in range(2)]
    kT = [P.sb("kT%d" % i, [128, 2, T], BF16) for i in range(2)]
    vv = [P.sb("vv%d" % i, [128, 16, 256], BF16) for i in range(2)]
    sg = [P.sb("sg%d" % i, [128, 16, 256], F32) for i in range(2)]
    yst = [P.sb("yst%d" % i, [128, 2, T], BF16) for i in range(2)]
    state = P.sb("state", [128, 2, 256], F32)
    state_bfs = [P.sb("state_bf%d" % i, [128, 2, 256], BF16) for i in range(2)]
    sTm = Rot([P.sb("sTm%d" % i, [128, 128], BF16) for i in range(2)])
    ocs = Rot([P.sb("ocs%d" % i, [128, 256], F32) for i in range(2)])
    osb = Rot([P.sb("osb%d" % i, [128, 256], F32) for i in range(8)])
    vdk = Rot([P.sb("vdk%d" % i, [128, 256], BF16) for i in range(2)])
    yb = Rot([P.sb("yb%d" % i, [128, 256], BF16) for i in range(8)])
    sq = P.sb("sqj", [128, 256], F32)
    st1 = Rot([P.sb("st1_%d" % i, [128, 4], F32) for i in range(8)])
    rotS = Rot(C.banks[0:2])
    rotOC = Rot(C.banks[2:4])
    rotU = Rot(C.banks[4:6])
    bankK = C.banks[6]
    bankT = C.banks[7]
    kkr = Rot([P.sb("kkr%d" % i, [128, 256], BF16) for i in range(3)])
    QRv = QRT.re("(hd p) t -> p hd t", p=128)
    KRv = KRT.re("(hd p) t -> p hd t", p=128)
    YRv = YRT.re("(hd p) t -> p hd t", p=128)
    VVv = VR.re("(n p) f -> p n f", p=128)
    GGv = GR.re("(n p) f -> p n f", p=128)
    for h in range(H):
        j = h % 2
        fs = slice(h * 256, (h + 1) * 256)
        P.dma("sp", qT[j], QRv[:, 2 * h:2 * h + 2, :], qT[j])
        P.dma("sp", kT[j], KRv[:, 2 * h:2 * h + 2, :], kT[j])
        P.dma("sp", vv[j], VVv[:, :, fs], vv[j])
        P.dma("sp", sg[j], GGv[:, :, fs], sg[j])
        g128 = math.exp(lgs[h] * 128.0)
        live = {}

        uls = {}

        def stage1a(n, h=h, j=j, uls=uls):
            cols = slice(n * 128, (n + 1) * 128)
            ps_s = rotS.next()
            for dc in range(2):
                P.mm(ps_s[:, 0:128], kT[j][:, dc, cols], qT[j][:, dc, cols], start=(dc == 0), stop=(dc == 1))
            ps_u = None
            if n < 15:
                pbk = psb(bankK).re("p (a t) -> p a t", a=8)
                for dc in range(2):
                    P.tr(pbk[:, dc, :], kT[j][:, dc, cols], C.ident)
                kkn = kkr.next()
                P.copy("act", kkn.re("p (a d) -> p a d", a=2), pbk[:, 0:2, :])
                vd = vdk.next()
                P.ts("pool", vd, vv[j][:, n, :], dk[:, h:h + 1], None, ALU.mult)
                ps_u = rotU.next()
                P.mm(ps_u[:, 0:256], kkn[:, 0:128], vd)
                P.mm(ps_u[:, 256:512], kkn[:, 128:256], vd)
            uls[n] = (ps_s, ps_u)

        def stage1b(n, h=h, j=j, g128=g128, live=live, uls=uls):
            cols = slice(n * 128, (n + 1) * 128)
            ps_s, ps_u = uls.pop(n)
            sm = sTm.next()
            P.tt("dve", sm, ps_s[:, 0:128], DT[:, h, :], ALU.mult)
            boc = rotOC.next()
            ps_o = boc[:, 0:256]
            P.mm(ps_o, sm, vv[j][:, n, :])
            ps_c = None
            if n > 0:
                ps_c = boc[:, 256:512]
                sbf = state_bfs[(n - 1) % 2]
                for dc in range(2):
                    P.mm(ps_c, qT[j][:, dc, cols], sbf[:, dc, :], start=(dc == 0), stop=(dc == 1))
            if n < 15:
                psu3 = ps_u.re("p (a e) -> p a e", a=2)
                if n == 0:
                    P.copy("dve", state, psu3)
                else:
                    P.stt("dve", state, state, g128, psu3, ALU.mult, ALU.add)
                P.copy("act", state_bfs[n % 2], state)
            live[n] = (ps_o, ps_c)

        obs = {}

        def combine(n, h=h, j=j, live=live, obs=obs):
            ps_o, ps_c = live.pop(n)
            ob = osb.next()
            if n > 0:
                oc = ocs.next()
                P.act(oc, ps_c, ACT.Identity, scale=dq[:, h:h + 1])
                P.tt("dve", ob, ps_o, oc, ALU.add)
            else:
                P.copy("dve", ob, ps_o)
            obs[n] = ob

        def norm_batch(ns, h=h, j=j, obs=obs):
            ob_ = [obs.pop(n) for n in ns]
            s1_ = [st1.next() for _ in ns]
            y_ = [yb.next() for _ in ns]
            for q in range(len(ns)):
                P.red(s1_[q][:, 0:1], ob_[q], ALU.add)
            for q in range(len(ns)):
                P.ts("dve", s1_[q][:, 1:2], s1_[q][:, 0:1], -1.0 / 256.0, None, ALU.mult)
            for q in range(len(ns)):
                P.ts("dve", ob_[q], ob_[q], s1_[q][:, 1:2], None, ALU.add)
            for q in range(len(ns)):
                P.act(sq, ob_[q], ACT.Square, accum=s1_[q][:, 2:3])
            for q in range(len(ns)):
                P.act(s1_[q][:, 2:3], s1_[q][:, 2:3], ACT.Sqrt, bias=EPS, scale=1.0 / 256.0)
            for q in range(len(ns)):
                P.recip(s1_[q][:, 3:4], s1_[q][:, 2:3])
            for q, n in enumerate(ns):
                P.stt("dve", y_[q], ob_[q], s1_[q][:, 3:4], sg[j][:, n, :], ALU.mult, ALU.mult)
            pb = psb(bankT).re("p (a t) -> p a t", a=8)
            for q in range(len(ns)):
                for dc in range(2):
                    P.tr(pb[:, 2 * q + dc, :], y_[q][:, dc * 128:(dc + 1) * 128], C.ident)
            for q, n in enumerate(ns):
                P.copy("act", yst[j][:, :, n * 128:(n + 1) * 128], pb[:, 2 * q:2 * q + 2, :])

        stage1a(0)
        for n in range(17):
            if n + 1 < 16:
                stage1a(n + 1)
            if n < 16:
                stage1b(n)
            if n >= 1:
                combine(n - 1)
                if (n - 1) % 4 == 3:
                    norm_batch(list(range(n - 4, n)))
        P.dma("sp", YRv[:, 2 * h:2 * h + 2, :], yst[j], yst[j])
    P.barrier()
    P.release(m)


def phase_nsa(C, QNT, KCT, VCT, KST, VS, KWT, VW, GN, cw, YNT):
    P = C.P
    m = P.mark()
    NH = 16
    SCALE = 128.0 ** -0.5
    BIG = 1.0e5
    BIGS = 30000.0
    slopes = [2.0 ** (-8.0 * (h + 1.0) / NH) for h in range(NH)]
    rotA = Rot(C.banks[0:4])
    obank = C.banks[4:8]

    def iota(v, pattern, base, cm):
        o = v.ap
        P.op("pool", lambda en: en.iota(o, pattern, base=base, channel_multiplier=cm, allow_small_or_imprecise_dtypes=True), writes=[v.buf])

    fregs = {}

    def asel(v, pattern, op, fill, base, cm):
        o = v.ap

        def f(en):
            if fill not in fregs:
                fregs[fill] = en.to_reg(fill)
            return en.affine_select(o, o, pattern, op, fregs[fill], base=base, channel_multiplier=cm)
        P.op("pool", f, reads=[v.buf], writes=[v.buf])

    BIASC = P.sb("BIASC", [128, NH, 127], F32)
    valc = P.sb("valc", [128, 127], F32)
    iota(valc, [[-16, 127]], 1889, 1)
    AL_T = P.sb("AL_T", [128, NH, 128], F32)
    AL_D = P.sb("AL_D", [128, NH, 128], F32)
    AL_W = P.sb("AL_W", [128, NH, 128], F32)
    vala = P.sb("vala", [128, 128], F32)
    iota(vala, [[1, 128]], 0, -1)
    for h in range(NH):
        P.ts("dve", BIASC[:, h, :], valc, -slopes[h], None, ALU.mult)
        asel(BIASC[:, h, :], [[-16, 127]], ALU.is_ge, -BIG, 1889, 1)
        P.ts("dve", AL_T[:, h, :], vala, -slopes[h], None, ALU.mult)
        P.copy("dve", AL_D[:, h, :], AL_T[:, h, :])
        asel(AL_D[:, h, :], [[1, 128]], ALU.is_ge, -BIG, 0, -1)
        P.copy("dve", AL_W[:, h, :], AL_T[:, h, :])
        asel(AL_W[:, h, :], [[-1, 128]], ALU.is_gt, -BIG, 0, 1)
    EALL = P.sb("EALL", [32, T], BF16)
    P.memset("pool", EALL, 1.0)
    asel(EALL, [[1, T]], ALU.is_ge, 0.0, 0, -64)
    asel(EALL, [[-1, T]], ALU.is_ge, 0.0, 63, 64)
    dcur = P.sb("dcur", [128, 64], F32)
    iota(dcur, [[1, 64]], -32, 0)
    pid = P.sb("pid", [128, 1], F32)
    iota(pid, [[0, 1]], 0, 1)
    P.ts("dve", pid, pid, 63.5, None, ALU.is_gt)
    P.ts("dve", dcur, dcur, pid[:, 0:1], None, ALU.subtract)
    KEEP = P.sb("KEEP", [128, 64], F32)
    ADDC = P.sb("ADDC", [128, 64], F32)
    tmpc = P.sb("tmpc", [128, 64], F32)
    P.ts("dve", KEEP, dcur, -1.5, None, ALU.is_lt)
    P.ts("dve", ADDC, dcur, -1.5, 1.0e4, ALU.is_gt, ALU.mult)
    P.ts("dve", tmpc, dcur, 0.5, -10001.0, ALU.is_gt, ALU.mult)
    P.tt("dve", ADDC, ADDC, tmpc, ALU.add)

    kcT = P.sb("kcT", [128, 4, 128], BF16)
    vc = P.sb("vc", [128, 4, 128], BF16)
    P.memset("pool", kcT, 0.0)
    P.memset("pool", vc, 0.0)
    KS = P.sb("KS", [128, 4, T], BF16)
    KW = P.sb("KW", [128, 4, T], BF16)
    VSs = P.sb("VSs", [128, 16, 4, 130], BF16)
    VWs = P.sb("VWs", [128, 16, 4, 130], BF16)
    P.memset("pool", VSs, 1.0)
    P.memset("pool", VWs, 1.0)
    P.dma("sp", KS, KST.re("(g p) t -> p g t", p=128), KS)
    P.dma("sp", KW, KWT.re("(g p) t -> p g t", p=128), KW)
    for c4 in range(16):
        P.dma("sp", VSs[:, c4, :, 0:128], VS.re("(c p) (g d) -> p c g d", p=128, g=4)[:, c4, :, :], VSs)
        P.dma("sp", VWs[:, c4, :, 0:128], VW.re("(c p) (g d) -> p c g d", p=128, g=4)[:, c4, :, :], VWs)

    m1 = P.mark()
    XC = P.sb("XC", [128, 4, T], BF16)
    W1 = P.sb("W1", [128, 32, 128], BF16)
    W2 = P.sb("W2", [128, 128], BF16)
    posr = P.sb("posr", [32, 128], F32)
    posT = P.sb("posT", [128, 32], BF16)
    cb = P.sb("cb", [128, 1], F32)
    hid = P.sb("hid", [128, 4, 127], BF16)
    for which, src in (("k", KCT), ("v", VCT)):
        P.dma("sp", XC, src.re("(g p) t -> p g t", p=128), XC)
        P.dma("pool", W1, cw["w1_" + which].re("(l d) h -> d l h", d=128), W1)
        P.dma("pool", W2, cw["w2_" + which], W2)
        P.dma("sp", posr, cw["pos_" + which], posr)
        b0 = rotA.next()
        P.tr(b0[:, 0:32], posr, C.identf[0:32, 0:32])
        P.copy("dve", posT, b0[:, 0:32])
        b1 = rotA.next()
        for l in range(32):
            P.mm(b1[:, 0:1], W1[:, l, :], posT[:, l:l + 1], start=(l == 0), stop=(l == 31))
        P.copy("dve", cb, b1[:, 0:1])
        b2 = rotA.next()
        b2v = b2[:, 0:508].re("p (g n) -> p g n", g=4)
        for l in range(32):
            P.mm(b2v, W1[:, l, :], XC[:, :, l:l + 2017:16], start=(l == 0), stop=(l == 31))
        P.act(hid, b2v, ACT.Silu, bias=cb[:, 0:1])
        b3 = rotA.next()
        if which == "k":
            b3v = b3[:, 0:508].re("p (g n) -> p g n", g=4)
            P.mm(b3v, W2, hid)
            P.copy("dve", kcT[:, :, 0:127], b3v)
        else:
            for g in range(4):
                P.mm(b3[0:127, g * 128:(g + 1) * 128], hid[:, g, :], W2)
            P.copy("dve", vc[0:127, :, :], b3[0:127, :].re("p (g d) -> p g d", g=4))
    P.barrier()
    P.release(m1)

    qt = [P.sb("qt%d" % i, [128, NH, 128], BF16) for i in range(2)]
    gnt = [P.sb("gnt%d" % i, [128, 48], F32) for i in range(2)]
    osum = [P.sb("osum%d" % i, [128, NH, 128], F32) for i in range(2)]
    ybn = [P.sb("ybn%d" % i, [128, NH * 128], BF16) for i in range(2)]
    ynst = [P.sb("ynst%d" % i, [128, NH, 128], BF16) for i in range(2)]
    s_sb = Rot([P.sb("s_sb%d" % i, [128, 4, 128], F32) for i in range(3)])
    pT = Rot([P.sb("pT%d" % i, [128, 4, 128], BF16) for i in range(3)])
    sc = Rot([P.sb("sc%d" % i, [128, 4, 128], F32) for i in range(2)])
    pe_ = Rot([P.sb("pe%d" % i, [128, 4, 128], F32) for i in range(2)])
    peb = Rot([P.sb("peb%d" % i, [128, 4, 128], BF16) for i in range(2)])
    pTc = Rot([P.sb("pTc%d" % i, [128, 4, 128], BF16) for i in range(2)])
    sm = Rot([P.sb("sm%d" % i, [128, 16], F32) for i in range(4)])
    Pimp = Rot([P.sb("Pimp%d" % i, [128, 128], F32) for i in range(2)])
    imp = Rot([P.sb("imp%d" % i, [128, 32], F32) for i in range(2)])
    cmp3 = P.sb("cmp3", [128, 32, 32], F32)
    cnt = Rot([P.sb("cnt%d" % i, [128, 32], F32) for i in range(2)])
    sbm = Rot([P.sb("sbm%d" % i, [128, 32], BF16) for i in range(2)])
    sbT = Rot([P.sb("sbT%d" % i, [32, 128], BF16) for i in range(2)])
    QNv = QNT.re("(h p) t -> p h t", p=128)
    YNv = YNT.re("(h p) t -> p h t", p=128)

    rotS = Rot(C.banks[0:3])
    rotC = Rot(C.banks[3:4])
    sma = Rot([P.sb("sma%d" % i, [128, 16], F32) for i in range(3)])

    def pull(bg, n):
        if bg is None:
            return
        for _ in range(n):
            try:
                next(bg)
            except StopIteration:
                return

    def chain(i, g, res):
        j = i % 2
        ncm = min(8 * i + 7, 127)
        off = 120 - 8 * i
        ps = rotC.next()
        psv = ps.re("p (r n) -> p r n", r=4)
        for r in range(4):
            P.mm(psv[:, r, 0:ncm], qt[j][:, 4 * g + r, :], kcT[:, g, 0:ncm])
        yield
        s = sc.next()
        P.stt("dve", s[:, :, 0:ncm], psv[:, :, 0:ncm], SCALE, BIASC[:, 4 * g:4 * g + 4, off:off + ncm], ALU.mult, ALU.add)
        yield
        st = sm.next()
        P.red(st[:, 0:4], s[:, :, 0:ncm], ALU.max)
        yield
        P.ts("dve", st[:, 0:4], st[:, 0:4], -1000.0, -1.0, ALU.max, ALU.mult)
        yield
        pp = pe_.next()
        for r in range(4):
            P.act(pp[:, r, 0:ncm], s[:, r, 0:ncm], ACT.Exp, bias=st[:, r:r + 1], accum=st[:, 4 + r:5 + r])
            yield
        P.ts("dve", st[:, 8:12], st[:, 4:8], 1e-30, None, ALU.add)
        yield
        P.recip(st[:, 8:12], st[:, 8:12])
        yield
        pb_ = peb.next()
        P.copy("pool", pb_[:, :, 0:ncm], pp[:, :, 0:ncm])
        yield
        bt = rotC.next()
        btv = psb(bt).re("p (r t) -> p r t", r=8)
        for r in range(4):
            P.tr(btv[0:ncm, r, :], pb_[:, r, 0:ncm], C.ident)
        yield
        ptc = pTc.next()
        P.copy("act", ptc[0:ncm, :, :], btv[0:ncm, 0:4, :])
        yield
        po = rotC.next()
        pov = po.re("p (r d) -> p r d", r=4)
        for r in range(4):
            P.mm(pov[:, r, :], ptc[0:ncm, r, :], vc[0:ncm, g, :])
        yield
        P.tt("dve", st[:, 12:16], st[:, 8:12], mkap(gnt[j][:, 12 * g:12 * g + 1], [[3, 4]]), ALU.mult)
        yield
        for r in range(4):
            P.ts("dve", osum[j][:, 4 * g + r, :], pov[:, r, :], st[:, 12 + r:13 + r], None, ALU.mult)
            yield
        sbt = None
        if i >= 8:
            Pi = Pimp.next()
            P.memset("pool", Pi, 0.0)
            P.ts("dve", Pi[:, 0:ncm], pp[:, 0, 0:ncm], st[:, 8:9], None, ALU.mult)
            yield
            for r in range(1, 4):
                P.stt("dve", Pi[:, 0:ncm], pp[:, r, 0:ncm], st[:, 8 + r:9 + r], Pi[:, 0:ncm], ALU.mult, ALU.add)
                yield
            im = imp.next()
            P.red(im, Pi.re("p (j f) -> p j f", f=4), ALU.add)
            yield
            P3 = mkap(Pi[:, 3:4], [[4, 32]])
            P.stt("dve", im, P3, -0.5, im, ALU.mult, ALU.add)
            yield
            P3s = mkap(Pi[:, 3:4], [[4, 31]])
            P.stt("dve", im[:, 1:32], P3s, 0.5, im[:, 1:32], ALU.mult, ALU.add)
            yield
            lo = 32 - 2 * i
            P.tt("dve", im, im, KEEP[:, lo:lo + 32], ALU.mult)
            yield
            P.tt("dve", im, im, ADDC[:, lo:lo + 32], ALU.add)
            yield
            P.memset("dve", im[:, 0:1], 1.0e4)
            yield
            inA = mkap(im[:, 0:1], [[0, 32], [1, 32]])
            inB = mkap(im[:, 0:1], [[1, 32], [0, 32]])
            P.tt("dve", cmp3, inA, inB, ALU.is_gt)
            yield
            cn = cnt.next()
            P.red(cn, cmp3, ALU.add)
            yield
            sb_ = sbm.next()
            P.ts("dve", sb_, cn, 15.5, -BIGS, ALU.is_ge, ALU.mult)
            yield
            bt2 = rotC.next()
            bt2v = psb(bt2)
            P.tr(bt2v[0:32, 0:128], sb_, C.ident)
            yield
            sbt = sbT.next()
            P.copy("act", sbt, bt2v[0:32, 0:128])
        res["sbt"] = sbt

    def load_tile(i):
        j = i % 2
        cols = slice(i * 128, (i + 1) * 128)
        P.dma("sp", qt[j], QNv[:, :, cols], qt[j])
        P.dma("sp", gnt[j], GN[cols, :], gnt[j])

    order = [(i, g) for i in range(16) for g in range(4)]
    items = []
    for (i, g) in order:
        for mode, Kt, Vt, gidx, chunks in (("sel", KS, VSs, 1, list(range(0, i + 1))),
                                            ("win", KW, VWs, 2, list(range(max(0, i - 4), i + 1)))):
            for ci, c in enumerate(chunks):
                items.append(dict(i=i, g=g, mode=mode, Kt=Kt, Vt=Vt, gidx=gidx, c=c, ci=ci, nch=len(chunks),
                                  first=(mode == "sel" and ci == 0), last_of_tile=(g == 3 and mode == "win" and ci == len(chunks) - 1)))
    chains = {}

    def get_chain(key):
        if key not in chains:
            res = {}
            chains[key] = [chain(key[0], key[1], res), res]
        return chains[key]

    def ensure_chain(key):
        gen, res = get_chain(key)
        for _ in gen:
            pass
        return res

    cur_bg = [None]

    def S1(it):
        i, g = it["i"], it["g"]
        j = i % 2
        if it["first"]:
            if g == 0 and i == 0:
                load_tile(0)
            if g == 1 and i + 1 < 16:
                load_tile(i + 1)
            ensure_chain((i, g))
            k = order.index((i, g))
            cur_bg[0] = get_chain(order[k + 1])[0] if k + 1 < len(order) else None
        sbt = chains[(i, g)][1].get("sbt")
        c = it["c"]
        ps = rotS.next()
        need_mask = (it["mode"] == "sel" and i >= 8 and c < i)
        P.mm(ps, it["Kt"][:, g, c * 128:(c + 1) * 128], qt[j][:, 4 * g:4 * g + 4, :], start=True, stop=not need_mask)
        if need_mask:
            for r in range(4):
                P.mm(ps[:, r * 128:(r + 1) * 128], EALL[:, c * 128:(c + 1) * 128], sbt, start=False, stop=(r == 3))
        it["ps"] = ps

    def S2(it):
        i, g, c = it["i"], it["g"], it["c"]
        d = i - c
        if d == 0:
            AL = AL_D
        elif it["mode"] == "win" and d == 4:
            AL = AL_W
        else:
            AL = AL_T
        s = s_sb.next()
        P.stt("dve", s, it["ps"].re("p (r t) -> p r t", r=4), SCALE, AL[:, 4 * g:4 * g + 4, :], ALU.mult, ALU.add)
        p_ = pT.next()
        for r in range(4):
            P.act(p_[:, r, :], s[:, r, :], ACT.Exp, bias=-slopes[4 * g + r] * 128.0 * d)
        it["p"] = p_

    def S3(it):
        i, g, c, ci, nch = it["i"], it["g"], it["c"], it["ci"], it["nch"]
        j = i % 2
        for r in range(4):
            P.mm(obank[r][:, 0:129], it["p"][:, r, :], it["Vt"][:, c, g, 0:129], start=(ci == 0), stop=(ci == nch - 1))
        pull(cur_bg[0], 4)
        if ci == nch - 1:
            s_ = sma.next()
            for r in range(4):
                h = 4 * g + r
                P.recip(s_[:, r:r + 1], obank[r][:, 128:129])
                P.tt("dve", s_[:, 4 + r:5 + r], s_[:, r:r + 1], gnt[j][:, 3 * h + it["gidx"]:3 * h + it["gidx"] + 1], ALU.mult)
                P.stt("dve", osum[j][:, h, :], obank[r][:, 0:128], s_[:, 4 + r:5 + r], osum[j][:, h, :], ALU.mult, ALU.add)
        if it["last_of_tile"]:
            cols = slice(i * 128, (i + 1) * 128)
            P.copy("pool", ybn[j], osum[j].re("p h d -> p (h d)"))
            for b in range(2):
                bank = rotC.next()
                pb = psb(bank).re("p (a t) -> p a t", a=8)
                for a in range(8):
                    hh = b * 8 + a
                    P.tr(pb[:, a, :], ybn[j][:, hh * 128:(hh + 1) * 128], C.ident)
                P.copy("act", ynst[j][:, b * 8:(b + 1) * 8, :], pb)
            P.dma("sp", YNv[:, :, cols], ynst[j], ynst[j])

    n_it = len(items)
    for k in range(n_it + 2):
        if k < n_it:
            S1(items[k])
        if 0 <= k - 1 < n_it:
            S2(items[k - 1])
        if 0 <= k - 2 < n_it:
            S3(items[k - 2])
    P.barrier()
    P.release(m)


def g1_blocks(S):
    b = []
    b += seg_blocks(0, 2048, "F", "bf16", S["QRT"])
    b += seg_blocks(2048, 2048, "F", "bf16", S["KRT"])
    b += seg_blocks(4096, 2048, "T", "bf16", S["VR"])
    b += seg_blocks(6144, 2048, "T", "silu32", S["GR"])
    b += seg_blocks(8192, 2048, "F", "bf16", S["QNT"])
    b += seg_blocks(10240, 512, "F", "bf16", S["KCT"])
    b += seg_blocks(10752, 512, "F", "bf16", S["VCT"])
    b += seg_blocks(11264, 512, "F", "bf16", S["KST"])
    b += seg_blocks(11776, 512, "T", "bf16", S["VS"])
    b += seg_blocks(12288, 512, "F", "bf16", S["KWT"])
    b += seg_blocks(12800, 512, "T", "bf16", S["VW"])
    b += seg_blocks(13312, 48, "T", "sig32", S["GN"])
    b += seg_blocks(13360, 4096, "F", "sig32", S["GRETT"])
    b += seg_blocks(17456, 4096, "F", "sig32", S["GNSAT"])
    return b


def build_program():
    nc = bass.Bass("TRN2", target_bir_lowering=False)
    C = Ctx(nc)
    x = C.inp("x", [T, D])
    I = {}
    for n, sh in [("mix_norm_pre", [NL, D]), ("w_in", [NL, D, DIN]), ("cmp_pos_k", [NL, 32, 128]), ("cmp_w1_k", [NL, 4096, 128]),
                  ("cmp_w2_k", [NL, 128, 128]), ("cmp_pos_v", [NL, 32, 128]), ("cmp_w1_v", [NL, 4096, 128]), ("cmp_w2_v", [NL, 128, 128]),
                  ("w_ret_up", [NL, 2048, D]), ("w_nsa_up", [NL, 2048, D]), ("w_out", [NL, D, D]), ("mix_norm_post", [NL, D]),
                  ("mlp_norm_pre", [NL, D]), ("w_mlp_in", [NL, D, DFF]), ("w_mlp_out", [NL, DFF, D]), ("mlp_norm_post", [NL, D])]:
        I[n] = C.inp(n, sh)
    ot = nc.dram_tensor("out", [T, D], F32, kind="ExternalOutput")
    out = V(ot.ap(), Buf("out"))
    S = {}
    for n, sh, dt in [("HT", [D, T], BF16), ("QRT", [2048, T], BF16), ("KRT", [2048, T], BF16), ("VR", [T, 2048], BF16), ("GR", [T, 2048], F32), ("QNT", [2048, T], BF16), ("KCT", [512, T], BF16),
                      ("VCT", [512, T], BF16), ("KST", [512, T], BF16), ("VS", [T, 512], BF16), ("KWT", [512, T], BF16),
                      ("VW", [T, 512], BF16), ("GN", [T, 48], F32), ("GRETT", [D, T], F32), ("GNSAT", [D, T], F32),
                      ("YRT", [2048, T], BF16), ("YNT", [2048, T], BF16), ("MT", [D, T], BF16), ("Z", [T, D], F32),
                      ("XA", [T, D], F32), ("XB", [T, D], F32), ("UT", [DFF, T], BF16), ("WB16", [DFF, D], BF16)]:
        S[n] = C.scratch(n, sh, dt)
    phase_nr_a(C, None, x, None, None, I["mix_norm_pre"][0], S["HT"])
    x_cur = x
    for l in range(NL):
        gemm(C, S["HT"], D, I["w_in"][l], g1_blocks(S), T)
        phase_ret(C, S["QRT"], S["KRT"], None, S["VR"], S["GR"], S["YRT"])
        cw = {"pos_k": I["cmp_pos_k"][l], "w1_k": I["cmp_w1_k"][l], "w2_k": I["cmp_w2_k"][l],
              "pos_v": I["cmp_pos_v"][l], "w1_v": I["cmp_w1_v"][l], "w2_v": I["cmp_w2_v"][l]}
        phase_nsa(C, S["QNT"], S["KCT"], S["VCT"], S["KST"], S["VS"], S["KWT"], S["VW"], S["GN"], cw, S["YNT"])
        phase_up(C, S["YRT"], S["YNT"], I["w_ret_up"][l], I["w_nsa_up"][l], S["GRETT"], S["GNSAT"], S["MT"])
        gemm(C, S["MT"], D, I["w_out"][l], seg_blocks(0, D, "T", "f32", S["Z"]), T)
        phase_nr_a(C, S["Z"], x_cur, S["XA"], I["mix_norm_post"][l], I["mlp_norm_pre"][l], S["HT"])
        x_cur = S["XA"]
        wo = I["w_mlp_out"][l]
        bgl = [(S["WB16"][r0:r0 + 512, :], wo[r0:r0 + 512, :]) for r0 in range(0, DFF, 512)]
        gemm(C, S["HT"], D, I["w_mlp_in"][l], seg_blocks(0, DFF, "F", "relu2", S["UT"]), T, bg=bgl)
        gemm(C, S["UT"], DFF, S["WB16"], seg_blocks(0, D, "T", "f32", S["Z"]), 512, KB=16, nwb=3)
        if l < NL - 1:
            phase_nr_a(C, S["Z"], x_cur, S["XB"], I["mlp_norm_post"][l], I["mix_norm_pre"][l + 1], S["HT"])
            x_cur = S["XB"]
        else:
            phase_nr_a(C, S["Z"], x_cur, out, I["mlp_norm_post"][l], None, None)
    C.P.emit()
    return nc


def kernel(**inputs):
    from concourse.bass_utils import run_bass_kernel_spmd
    nc = build_program()
    n = 8
    x = np.ascontiguousarray(np.asarray(inputs["x"], dtype=np.float32))
    shared = {k: np.ascontiguousarray(np.asarray(v, dtype=np.float32)) for k, v in inputs.items() if k != "x"}
    in_maps = []
    for b in range(n):
        mp = dict(shared)
        mp["x"] = x[b]
        in_maps.append(mp)
    res = run_bass_kernel_spmd(nc, in_maps, core_ids=list(range(n)))
    return np.stack([np.asarray(r["out"], dtype=np.float32) for r in res.results], axis=0)
```

```python
import numpy as np
import concourse.bass as bass
import concourse.mybir as mybir

F32 = mybir.dt.float32
BF16 = mybir.dt.bfloat16
I32 = mybir.dt.int32
ACT = mybir.ActivationFunctionType
ALU = mybir.AluOpType
AX = mybir.AxisListType


class TL:
    __slots__ = ("name", "step", "n", "marks", "handle", "cmap", "phys", "base")

    def __init__(self, name, step):
        self.name, self.step, self.n, self.marks, self.handle, self.cmap = name, step, 0, set(), None, None
        self.phys, self.base = None, 0


class PhysSem:
    __slots__ = ("name", "total", "handle")

    def __init__(self, name):
        self.name, self.total, self.handle = name, 0, None


class Buf:
    __slots__ = ("name", "last_w", "readers", "tl")

    def __init__(self, name):
        self.name, self.last_w, self.readers, self.tl = name, None, {}, None


class V:
    __slots__ = ("ap", "buf")

    def __init__(self, ap, buf):
        self.ap, self.buf = ap, buf

    def __getitem__(self, idx):
        return V(self.ap[idx], self.buf)

    def re(self, pat, **kw):
        return V(self.ap.rearrange(pat, **kw), self.buf)


class Prog:
    ENGS = ("pe", "act", "dve", "pool", "sp")

    def __init__(self, nc):
        self.nc = nc
        self.tl = {e: TL(e, 1) for e in self.ENGS}
        self.seen = {e: {} for e in self.ENGS}
        self.ops = {e: [] for e in self.ENGS}
        self.dma_tls = []
        self.stack = []
        self.nbuf = 0
        self.free_phys = []
        self.all_phys = []
        for e in self.ENGS:
            ph = PhysSem("eng_" + e)
            self.all_phys.append(ph)
            self.tl[e].phys = ph

    def sb(self, name, shape, dt):
        g = self.nc.sbuf_tensor(name + "_%d" % self.nbuf, list(shape), dt)
        self.nbuf += 1
        t = g.__enter__()
        self.stack.append(g)
        return V(t[:] if not hasattr(t, "ap") else t.ap(), Buf(name))

    def ps(self, name, shape, dt=F32):
        g = self.nc.psum_tensor(name + "_%d" % self.nbuf, list(shape), dt)
        self.nbuf += 1
        t = g.__enter__()
        self.stack.append(g)
        return V(t[:] if not hasattr(t, "ap") else t.ap(), Buf(name))

    def mark(self):
        return (len(self.stack), len(self.dma_tls))

    def release(self, mark):
        ns, nt = mark
        while len(self.stack) > ns:
            self.stack.pop().__exit__(None, None, None)
        for t in self.dma_tls[nt:]:
            if t.phys is not None:
                t.phys.total += len(t.marks) * t.step
                self.free_phys.append(t.phys)
                t.phys = None
                t.cmap = "retired"
        self.retired = getattr(self, "retired", [])
        self.retired.extend(self.dma_tls[nt:])
        del self.dma_tls[nt:]

    def dma_tl(self, name):
        t = TL(name, 16)
        if self.free_phys:
            t.phys = self.free_phys.pop()
        else:
            t.phys = PhysSem("dsem%d" % len(self.all_phys))
            self.all_phys.append(t.phys)
        t.base = t.phys.total
        t.handle = t.phys
        self.dma_tls.append(t)
        return t

    def op(self, eng, fn, reads=(), writes=(), tl=None, extra_deps=()):
        own = self.tl[eng]
        tline = tl or own
        seen = self.seen[eng]
        deps = list(extra_deps)
        for b in reads:
            if b.last_w is not None:
                deps.append(b.last_w)
        for b in writes:
            if b.last_w is not None and b.last_w[0] is not own:
                deps.append(b.last_w)
            for s, i in b.readers.items():
                if s is not own:
                    deps.append((s, i))
        waits = {}
        for s, i in deps:
            if s is own:
                if eng == "pe" or eng == "sp":
                    continue
            if seen.get(s, 0) >= i:
                continue
            if waits.get(s, 0) < i:
                waits[s] = i
        for s, i in waits.items():
            seen[s] = i
            s.marks.add(i)
        tline.n += 1
        idx = tline.n
        self.ops[eng].append((fn, list(waits.items()), (tline, idx)))
        for b in reads:
            if b.readers.get(tline, 0) < idx:
                b.readers[tline] = idx
        for b in writes:
            b.last_w = (tline, idx)
            b.readers = {}
        return (tline, idx)

    def barrier(self):
        toks = []
        for e in self.ENGS:
            deps = []
            if self.tl[e].n > 0:
                deps.append((self.tl[e], self.tl[e].n))
            if e == "sp":
                for t in self.dma_tls:
                    if t.n > 0:
                        deps.append((t, t.n))
            own = self.tl[e]
            waits = {}
            for s, i in deps:
                if self.seen[e].get(s, 0) >= i and s is not own:
                    continue
                waits[s] = i
                s.marks.add(i)
                self.seen[e][s] = i
            own.n += 1
            idx = own.n
            self.ops[e].append((lambda en: en.nop(), list(waits.items()), (own, idx)))
            toks.append((own, idx))
        for e in self.ENGS:
            waits = {}
            for s, i in toks:
                if s is self.tl[e]:
                    continue
                waits[s] = i
                s.marks.add(i)
                self.seen[e][s] = i
            for t in self.dma_tls:
                self.seen[e][t] = t.n
            own = self.tl[e]
            own.n += 1
            self.ops[e].append((lambda en: en.nop(), list(waits.items()), (own, own.n)))

    def emit(self):
        nc = self.nc
        guards = []
        for ph in self.all_phys:
            g = nc.semaphore(ph.name)
            ph.handle = g.__enter__()
            guards.append(g)
        tls = list(self.tl.values()) + self.dma_tls + getattr(self, "retired", [])
        for t in tls:
            ph = t.handle if isinstance(t.handle, PhysSem) else t.phys
            t.handle = ph.handle
            ms = sorted(t.marks)
            t.cmap = {m: t.base + (k + 1) * t.step for k, m in enumerate(ms)}
        print("semaphores used:", len(self.all_phys), "ops:", {e: len(v) for e, v in self.ops.items()})
        engmap = {"pe": "tensor", "act": "scalar", "dve": "vector", "pool": "gpsimd", "sp": "sync"}
        with nc.Block() as block:
            for e in self.ENGS:
                ops = self.ops[e]

                def body(en, ops=ops):
                    for fn, waits, (tline, idx) in ops:
                        for s, i in waits:
                            en.wait_ge(s.handle, s.cmap[i])
                        ins = fn(en)
                        if idx in tline.marks:
                            ins.then_inc(tline.handle, tline.step)
                getattr(block, engmap[e])(body)
        for g in reversed(guards):
            g.__exit__(None, None, None)


    @staticmethod
    def _a(x):
        return x.ap if isinstance(x, V) else x

    @staticmethod
    def _b(*xs):
        return [x.buf for x in xs if isinstance(x, V)]

    def dma(self, q, out, in_, side):
        b = side.buf
        if b.tl is None:
            b.tl = self.dma_tl("d_" + b.name)
        o, i = out.ap, in_.ap
        return self.op(q, lambda en: en.dma_start(out=o, in_=i), reads=[in_.buf], writes=[out.buf], tl=b.tl)

    def mm(self, out, lhsT, rhs, start=True, stop=True):
        o, l, r = out.ap, lhsT.ap, rhs.ap
        return self.op("pe", lambda en: en.matmul(o, l, r, start=start, stop=stop),
                       reads=[lhsT.buf, rhs.buf], writes=[out.buf])

    def tr(self, out, in_, ident):
        o, i, d = out.ap, in_.ap, ident.ap
        return self.op("pe", lambda en: en.transpose(o, i, d), reads=[in_.buf, ident.buf], writes=[out.buf])

    def act(self, out, in_, func, bias=0.0, scale=1.0, accum=None):
        o, i, b, s = out.ap, in_.ap, self._a(bias), self._a(scale)
        ac = accum.ap if accum is not None else None
        w = [out.buf] + ([accum.buf] if accum is not None else [])
        if ac is None:
            f = lambda en: en.activation(out=o, in_=i, func=func, bias=b, scale=s)
        else:
            f = lambda en: en.activation(out=o, in_=i, func=func, bias=b, scale=s, accum_out=ac)
        return self.op("act", f, reads=[in_.buf] + self._b(bias, scale), writes=w)

    def ts(self, eng, out, in0, s1, s2, op0, op1=None):
        o, i, a1, a2 = out.ap, in0.ap, self._a(s1), self._a(s2)
        if op1 is None:
            f = lambda en: en.tensor_scalar(o, i, a1, None, op0)
        else:
            f = lambda en: en.tensor_scalar(o, i, a1, a2, op0, op1)
        return self.op(eng, f, reads=[in0.buf] + self._b(s1, s2), writes=[out.buf])

    def tt(self, eng, out, in0, in1, op):
        o, a, b = out.ap, in0.ap, in1.ap
        return self.op(eng, lambda en: en.tensor_tensor(o, a, b, op), reads=[in0.buf, in1.buf], writes=[out.buf])

    def stt(self, eng, out, in0, scalar, in1, op0, op1):
        o, a, s, b = out.ap, in0.ap, self._a(scalar), in1.ap
        return self.op(eng, lambda en: en.scalar_tensor_tensor(o, a, s, b, op0, op1),
                       reads=[in0.buf, in1.buf] + self._b(scalar), writes=[out.buf])

    def red(self, out, in_, op, axis=None):
        o, i = out.ap, in_.ap
        ax = axis or AX.X
        return self.op("dve", lambda en: en.tensor_reduce(o, i, ax, op), reads=[in_.buf], writes=[out.buf])

    def copy(self, eng, out, in_):
        o, i = out.ap, in_.ap
        if eng == "act":
            f = lambda en: en.copy(o, i)
        else:
            f = lambda en: en.tensor_copy(o, i)
        return self.op(eng, f, reads=[in_.buf], writes=[out.buf])

    def memset(self, eng, out, val):
        o = out.ap
        return self.op(eng, lambda en: en.memset(o, val), writes=[out.buf])

    def recip(self, out, in_):
        o, i = out.ap, in_.ap
        return self.op("dve", lambda en: en.reciprocal(o, i), reads=[in_.buf], writes=[out.buf])


import math

T = 2048
D = 4096
DIN = 21552
DFF = 16384
EPS = 1e-6
NL = 2


class Rot:
    def __init__(self, items):
        self.items, self.i = items, 0

    def next(self):
        x = self.items[self.i % len(self.items)]
        self.i += 1
        return x


class Ctx:
    def __init__(self, nc, dbg=()):
        self.nc = nc
        self.P = Prog(nc)
        self.dbg = set(dbg)
        self.dram = {}
        self.wbuf = Buf("weights")
        P = self.P
        self.banks = [P.ps("bank%d" % i, [128, 512], F32) for i in range(8)]
        self.bank_rot = Rot(self.banks)
        self.ident = P.sb("ident", [128, 128], BF16)
        self.identf = P.sb("identf", [128, 128], F32)
        for idt in (self.ident, self.identf):
            P.memset("pool", idt, 0.0)
            o = idt.ap
            P.op("pool", lambda en, o=o: en.affine_select(o, o, [[-1, 128]], ALU.not_equal, 1.0, base=0, channel_multiplier=1),
                 reads=[idt.buf], writes=[idt.buf])
        self.altc = 0

    def next_ps(self):
        return self.bank_rot.next()

    def alt(self):
        self.altc += 1
        return "act" if self.altc % 2 else "dve"

    def inp(self, name, shape, dt=F32):
        t = self.nc.dram_tensor(name, list(shape), dt, kind="ExternalInput")
        v = V(t.ap(), self.wbuf)
        self.dram[name] = v
        return v

    def scratch(self, name, shape, dt):
        kind = "ExternalOutput" if name in self.dbg else "Internal"
        t = self.nc.dram_tensor(name, list(shape), dt, kind=kind)
        v = V(t.ap(), Buf(name))
        self.dram[name] = v
        return v


def psb(v):
    return V(v.ap.bitcast(BF16), v.buf)


def phase_nr_a(C, z, x_src, x_dst, g_post, g_pre, HT):
    P = C.P
    m = P.mark()
    need_nr = z is not None
    need_a = HT is not None
    xt = [P.sb("xt%d" % i, [128, D], F32) for i in range(3)]
    junk = P.sb("junk", [128, D], BF16)
    junk2 = P.sb("junk2", [128, D], BF16)
    if need_nr:
        zt = [P.sb("zt%d" % i, [128, D], F32) for i in range(3)]
        gpost = P.sb("gpost", [128, D], F32)
        P.dma("sp", gpost, V(g_post.ap.partition_broadcast(128), g_post.buf), gpost)
        ssz = [P.sb("ssz%d" % i, [128, 1], F32) for i in range(2)]
        rz = [P.sb("rz%d" % i, [128, 1], F32) for i in range(2)]
    if need_a:
        gpre = P.sb("gpre", [128, D], F32)
        P.dma("sp", gpre, V(g_pre.ap.partition_broadcast(128), g_pre.buf), gpre)
        hb = [P.sb("hb%d" % i, [128, D], BF16) for i in range(2)]
        hs = [P.sb("hs%d" % i, [128, 32, 256], BF16) for i in range(2)]
        ssx = [P.sb("ssx%d" % i, [128, 1], F32) for i in range(2)]
        rx = [P.sb("rx%d" % i, [128, 1], F32) for i in range(2)]
        HTv = HT.re("(kc p) t -> p kc t", p=128)
    def stage1(i):
        j = i % 2
        j3 = i % 3
        rows = slice(i * 128, (i + 1) * 128)
        P.dma("sp", xt[j3], x_src[rows, :], xt[j3])
        if need_nr:
            P.dma("sp", zt[j3], z[rows, :], zt[j3])
            P.act(junk, zt[j3], ACT.Square, accum=ssz[j])
            P.act(ssz[j], ssz[j], ACT.Sqrt, bias=EPS, scale=1.0 / D)
            P.recip(rz[j], ssz[j])
            P.stt("dve", zt[j3], zt[j3], rz[j][:, 0:1], gpost, ALU.mult, ALU.mult)
            P.tt("pool", xt[j3], xt[j3], zt[j3], ALU.add)
            P.dma("pool", x_dst[rows, :], xt[j3], xt[j3])

    def stage2(i):
        j = i % 2
        j3 = i % 3
        if need_a:
            P.act(junk2, xt[j3], ACT.Square, accum=ssx[j])
            P.act(ssx[j], ssx[j], ACT.Sqrt, bias=EPS, scale=1.0 / D)
            P.recip(rx[j], ssx[j])
            P.stt("dve", hb[j], xt[j3], rx[j][:, 0:1], gpre, ALU.mult, ALU.mult)
            hsl = hs[(i // 2) % 2]
            half = (i % 2) * 128
            for b in range(4):
                bank = C.next_ps()
                pb = psb(bank).re("p (a t) -> p a t", a=8)
                for a in range(8):
                    kc = b * 8 + a
                    P.tr(pb[:, a, :], hb[j][:, kc * 128:(kc + 1) * 128], C.ident)
                P.copy(C.alt(), hsl[:, b * 8:(b + 1) * 8, half:half + 128], pb)
            if i % 2 == 1:
                t0 = (i - 1) * 128
                for h2 in range(2):
                    P.dma("act", HTv[:, h2 * 16:(h2 + 1) * 16, t0:t0 + 256], hsl[:, h2 * 16:(h2 + 1) * 16, :], hsl)

    nt = T // 128
    for k in range(nt + 1):
        if k < nt:
            stage1(k)
        if k >= 1:
            stage2(k - 1)
    P.barrier()
    P.release(m)


class Epi:
    def __init__(self, C, st32, st16):
        self.C, self.st32, self.st16 = C, st32, st16

    def make(self, kind, dst, seg0, orient):
        C, P = self.C, self.C.P

        def epi(ps, w, a0, b0):
            if orient == "F":
                src = ps[0:w, :]
                sl = lambda st: st[0:w, :]
                d = dst[a0 - seg0:a0 - seg0 + w, b0:b0 + 512]
            else:
                src = ps[:, 0:w]
                sl = lambda st: st[:, 0:w]
                d = dst[a0:a0 + 128, b0 - seg0:b0 - seg0 + w]
            if kind == "bf16":
                st = self.st16.next()
                P.copy(C.alt(), sl(st), src)
            elif kind == "f32":
                st = self.st32.next()
                P.copy(C.alt(), sl(st), src)
            elif kind == "sig32":
                st = self.st32.next()
                P.act(sl(st), src, ACT.Sigmoid)
            elif kind == "silu32":
                st = self.st32.next()
                P.act(sl(st), src, ACT.Silu)
            elif kind == "relu2":
                tmp = self.st32.next()
                st = self.st16.next()
                P.act(sl(tmp), src, ACT.Relu)
                P.tt("dve", sl(st), sl(tmp), sl(tmp), ALU.mult)
            else:
                raise ValueError(kind)
            P.dma("sp", d, sl(st), st)
        return epi


def gemm(C, src, K, W, blocks, TT, KB=32, nwb=2, bg=None):
    P = C.P
    m = P.mark()
    bg = list(bg or [])
    bgside = V(None, Buf("bgcast"))

    def bg_issue(n=1):
        for _ in range(n):
            if bg:
                o, i = bg.pop(0)
                P.dma("pool", o, i, bgside)
    KC = K // 128
    KB = min(KC, KB)
    NKB = KC // KB
    A_full = P.sb("A", [128, KC, TT], BF16)
    APW = 8
    A_parts = [V(A_full.ap[:, k0:k0 + APW, :], Buf("A_%d" % k0)) for k0 in range(0, KC, APW)]

    class _A:
        def __getitem__(self, idx):
            p_, kc, tsl = idx
            return A_parts[kc // APW][p_, kc % APW, tsl]
    A = _A()
    wb = Rot([P.sb("wb%d" % i, [128, KB, 512], BF16) for i in range(nwb)])
    st32 = Rot([P.sb("st32_%d" % i, [128, 512], F32) for i in range(3)])
    st16 = Rot([P.sb("st16_%d" % i, [128, 512], BF16) for i in range(3)])
    E = Epi(C, st32, st16)
    Wv = W.re("(kc p) n -> p kc n", p=128)
    srcv = src.re("(kc p) t -> p kc t", p=128)
    for t0 in range(0, T, TT):
        for ip, k0 in enumerate(range(0, KC, APW)):
            P.dma("sp", A_parts[ip], srcv[:, k0:k0 + APW, t0:t0 + TT], A_parts[ip])
        for (c0, ncols, orient, kind, dst, seg0) in blocks:
            epi = E.make(kind, dst, seg0, orient)
            if orient == "F":
                assert NKB == 1
                w = wb.next()
                P.dma("pool", w[:, :, 0:ncols], Wv[:, :, c0:c0 + ncols], w)
                bg_issue(1)
                for fc in range(0, ncols, 128):
                    fw = min(128, ncols - fc)
                    for tg in range(0, TT, 512):
                        ps = C.next_ps()
                        for kc in range(KC):
                            P.mm(ps[0:fw, :], w[:, kc, fc:fc + fw], A[:, kc, tg:tg + 512], start=(kc == 0), stop=(kc == KC - 1))
                        epi(ps, fw, c0 + fc, t0 + tg)
            else:
                ntt = TT // 128
                if NKB == 1:
                    w = wb.next()
                    P.dma("pool", w[:, :, 0:ncols], Wv[:, :, c0:c0 + ncols], w)
                    for tt in range(ntt):
                        ps = C.next_ps()
                        for kc in range(KC):
                            P.mm(ps[:, 0:ncols], A[:, kc, tt * 128:(tt + 1) * 128], w[:, kc, 0:ncols], start=(kc == 0), stop=(kc == KC - 1))
                        epi(ps, ncols, t0 + tt * 128, c0)
                else:
                    pss = [C.next_ps() for _ in range(ntt)]
                    for kb in range(NKB):
                        w = wb.next()
                        P.dma("pool", w[:, :, 0:ncols], Wv[:, kb * KB:(kb + 1) * KB, c0:c0 + ncols], w)
                        for tt in range(ntt):
                            for kc in range(KB):
                                P.mm(pss[tt][:, 0:ncols], A[:, kb * KB + kc, tt * 128:(tt + 1) * 128], w[:, kc, 0:ncols],
                                     start=(kb == 0 and kc == 0), stop=(kb == NKB - 1 and kc == KB - 1))
                    for tt in range(ntt):
                        epi(pss[tt], ncols, t0 + tt * 128, c0)
    bg_issue(len(bg))
    P.barrier()
    P.release(m)


def seg_blocks(c0, n, orient, kind, dst, bw=512):
    out = []
    for c in range(c0, c0 + n, bw):
        out.append((c, min(bw, c0 + n - c), orient, kind, dst, c0))
    return out


def phase_up(C, YRT, YNT, Wr, Wn, GRETT, GNSAT, MT):
    P = C.P
    m = P.mark()
    TT = T
    BW = 256
    Ar = P.sb("Ar", [128, 16, TT], BF16)
    An = P.sb("An", [128, 16, TT], BF16)
    wr = Rot([P.sb("wr%d" % i, [128, 16, BW], BF16) for i in range(2)])
    wn = Rot([P.sb("wn%d" % i, [128, 16, BW], BF16) for i in range(2)])
    grt = Rot([P.sb("grt%d" % i, [128, 512], F32) for i in range(4)])
    gnt = Rot([P.sb("gnt%d" % i, [128, 512], F32) for i in range(4)])
    tmp = Rot([P.sb("tmp%d" % i, [128, 512], F32) for i in range(3)])
    st16 = Rot([P.sb("st16u_%d" % i, [128, 512], BF16) for i in range(3)])
    Wrv = Wr.re("(kc p) n -> p kc n", p=128)
    Wnv = Wn.re("(kc p) n -> p kc n", p=128)
    yrv = YRT.re("(kc p) t -> p kc t", p=128)
    ynv = YNT.re("(kc p) t -> p kc t", p=128)
    Ar_p = [V(Ar.ap[:, k0:k0 + 4, :], Buf("Ar_%d" % k0)) for k0 in range(0, 16, 4)]
    An_p = [V(An.ap[:, k0:k0 + 4, :], Buf("An_%d" % k0)) for k0 in range(0, 16, 4)]
    for ip, k0 in enumerate(range(0, 16, 4)):
        P.dma("sp", Ar_p[ip], yrv[:, k0:k0 + 4, :], Ar_p[ip])
        P.dma("sp", An_p[ip], ynv[:, k0:k0 + 4, :], An_p[ip])
    for nb in range(D // BW):
        a, b = wr.next(), wn.next()
        P.dma("pool", a, Wrv[:, :, nb * BW:(nb + 1) * BW], a)
        P.dma("pool", b, Wnv[:, :, nb * BW:(nb + 1) * BW], b)
        for fc in range(BW // 128):
            f0 = nb * BW + fc * 128
            for tg in range(0, TT, 512):
                psr, psn = C.next_ps(), C.next_ps()
                for kc in range(16):
                    P.mm(psr, a[:, kc, fc * 128:(fc + 1) * 128], Ar_p[kc // 4][:, kc % 4, tg:tg + 512], start=(kc == 0), stop=(kc == 15))
                for kc in range(16):
                    P.mm(psn, b[:, kc, fc * 128:(fc + 1) * 128], An_p[kc // 4][:, kc % 4, tg:tg + 512], start=(kc == 0), stop=(kc == 15))
                g1, g2, t1, st = grt.next(), gnt.next(), tmp.next(), st16.next()
                tok = slice(tg, tg + 512)
                P.dma("act", g1, GRETT[f0:f0 + 128, tok], g1)
                P.dma("act", g2, GNSAT[f0:f0 + 128, tok], g2)
                P.tt("dve", g1, psr, g1, ALU.mult)
                P.tt("dve", t1, psn, g2, ALU.mult)
                P.tt("pool", st, t1, g1, ALU.add)
                P.dma("sp", MT[f0:f0 + 128, tok], st, st)
    P.barrier()
    P.release(m)


def mkap(v, dims):
    a = v.ap
    return V(bass.AP(a.tensor, a.offset, [list(a.ap[0])] + [list(d) for d in dims]), v.buf)


def phase_ret(C, QRT, KRT, KR, VR, GR, YRT):
    P = C.P
    m = P.mark()
    H = 8
    lgs = [math.log1p(-2.0 ** (-5.0 - h)) for h in range(H)]
    LN16 = math.log(1.0 / 16.0)
    val = P.sb("val", [128, 128], F32)
    o = val.ap
    P.op("pool", lambda en: en.iota(o, [[1, 128]], base=0, channel_multiplier=-1, allow_small_or_imprecise_dtypes=True), writes=[val.buf])
    P.ts("pool", val, val, 0.0, None, ALU.max)
    pidx = P.sb("pidx", [128, 1], F32)
    o2 = pidx.ap
    P.op("pool", lambda en: en.iota(o2, [[0, 1]], base=0, channel_multiplier=1, allow_small_or_imprecise_dtypes=True), writes=[pidx.buf])
    DT = P.sb("DT", [128, H, 128], F32)
    dq = P.sb("dq", [128, H], F32)
    dk = P.sb("dk", [128, H], F32)
    for h in range(H):
        P.act(DT[:, h, :], val, ACT.Exp, bias=LN16, scale=lgs[h])
        oo = DT[:, h, :].ap
        P.op("pool", lambda en, oo=oo: en.affine_select(oo, oo, [[1, 128]], ALU.is_ge, 0.0, base=0, channel_multiplier=-1),
             reads=[DT.buf], writes=[DT.buf])
        P.act(dq[:, h:h + 1], pidx, ACT.Exp, bias=lgs[h], scale=lgs[h])
        P.act(dk[:, h:h + 1], pidx, ACT.Exp, bias=127.0 * lgs[h] + LN16, scale=-lgs[h])
    qT = [P.sb("qT%d" % i, [128, 2, T], BF16) for i in range(2)]
    kT = [P.sb("kT%d" % i, [128, 2, T], BF16) for i in range(2)]
    vv = [P.sb("vv%d" % i, [128, 16, 256], BF16) for i in range(2)]
    sg = [P.sb("sg%d" % i, [128, 16, 256], F32) for i in range(2)]
    yst = [P.sb("yst%d" % i, [128, 2, T], BF16) for i in range(2)]
    state = P.sb("state", [128, 2, 256], F32)
    state_bfs = [P.sb("state_bf%d" % i, [128, 2, 256], BF16) for i in range(2)]
    sTm = Rot([P.sb("sTm%d" % i, [128, 128], BF16) for i in range(2)])
    ocs = Rot([P.sb("ocs%d" % i, [128, 256], F32) for i in range(2)])
    osb = Rot([P.sb("osb%d" % i, [128, 256], F32) for i in range(8)])
    vdk = Rot([P.sb("vdk%d" % i, [128, 256], BF16) for i in range(2)])
    yb = Rot([P.sb("yb%d" % i, [128, 256], BF16) for i in range(8)])
    sq = P.sb("sqj", [128, 256], F32)
    st1 = Rot([P.sb("st1_%d" % i, [128, 4], F32) for i in range(8)])
    rotS = Rot(C.banks[0:2])
    rotOC = Rot(C.banks[2:4])
    rotU = Rot(C.banks[4:6])
    bankK = C.banks[6]
    bankT = C.banks[7]
    kkr = Rot([P.sb("kkr%d" % i, [128, 256], BF16) for i in range(3)])
    QRv = QRT.re("(hd p) t -> p hd t", p=128)
    KRv = KRT.re("(hd p) t -> p hd t", p=128)
    YRv = YRT.re("(hd p) t -> p hd t", p=128)
    VVv = VR.re("(n p) f -> p n f", p=128)
    GGv = GR.re("(n p) f -> p n f", p=128)
    for h in range(H):
        j = h % 2
        fs = slice(h * 256, (h + 1) * 256)
        P.dma("sp", qT[j], QRv[:, 2 * h:2 * h + 2, :], qT[j])
        P.dma("sp", kT[j], KRv[:, 2 * h:2 * h + 2, :], kT[j])
        P.dma("sp", vv[j], VVv[:, :, fs], vv[j])
        P.dma("sp", sg[j], GGv[:, :, fs], sg[j])
        g128 = math.exp(lgs[h] * 128.0)
        live = {}

        uls = {}

        def stage1a(n, h=h, j=j, uls=uls):
            cols = slice(n * 128, (n + 1) * 128)
            ps_s = rotS.next()
            for dc in range(2):
                P.mm(ps_s[:, 0:128], kT[j][:, dc, cols], qT[j][:, dc, cols], start=(dc == 0), stop=(dc == 1))
            ps_u = None
            if n < 15:
                pbk = psb(bankK).re("p (a t) -> p a t", a=8)
                for dc in range(2):
                    P.tr(pbk[:, dc, :], kT[j][:, dc, cols], C.ident)
                kkn = kkr.next()
                P.copy("act", kkn.re("p (a d) -> p a d", a=2), pbk[:, 0:2, :])
                vd = vdk.next()
                P.ts("pool", vd, vv[j][:, n, :], dk[:, h:h + 1], None, ALU.mult)
                ps_u = rotU.next()
                P.mm(ps_u[:, 0:256], kkn[:, 0:128], vd)
                P.mm(ps_u[:, 256:512], kkn[:, 128:256], vd)
            uls[n] = (ps_s, ps_u)

        def stage1b(n, h=h, j=j, g128=g128, live=live, uls=uls):
            cols = slice(n * 128, (n + 1) * 128)
            ps_s, ps_u = uls.pop(n)
            sm = sTm.next()
            P.tt("dve", sm, ps_s[:, 0:128], DT[:, h, :], ALU.mult)
            boc = rotOC.next()
            ps_o = boc[:, 0:256]
            P.mm(ps_o, sm, vv[j][:, n, :])
            ps_c = None
            if n > 0:
                ps_c = boc[:, 256:512]
                sbf = state_bfs[(n - 1) % 2]
                for dc in range(2):
                    P.mm(ps_c, qT[j][:, dc, cols], sbf[:, dc, :], start=(dc == 0), stop=(dc == 1))
            if n < 15:
                psu3 = ps_u.re("p (a e) -> p a e", a=2)
                if n == 0:
                    P.copy("dve", state, psu3)
                else:
                    P.stt("dve", state, state, g128, psu3, ALU.mult, ALU.add)
                P.copy("act", state_bfs[n % 2], state)
            live[n] = (ps_o, ps_c)

        obs = {}

        def combine(n, h=h, j=j, live=live, obs=obs):
            ps_o, ps_c = live.pop(n)
            ob = osb.next()
            if n > 0:
                oc = ocs.next()
                P.act(oc, ps_c, ACT.Identity, scale=dq[:, h:h + 1])
                P.tt("dve", ob, ps_o, oc, ALU.add)
            else:
                P.copy("dve", ob, ps_o)
            obs[n] = ob

        def norm_batch(ns, h=h, j=j, obs=obs):
            ob_ = [obs.pop(n) for n in ns]
            s1_ = [st1.next() for _ in ns]
            y_ = [yb.next() for _ in ns]
            for q in range(len(ns)):
                P.red(s1_[q][:, 0:1], ob_[q], ALU.add)
            for q in range(len(ns)):
                P.ts("dve", s1_[q][:, 1:2], s1_[q][:, 0:1], -1.0 / 256.0, None, ALU.mult)
            for q in range(len(ns)):
                P.ts("dve", ob_[q], ob_[q], s1_[q][:, 1:2], None, ALU.add)
            for q in range(len(ns)):
                P.act(sq, ob_[q], ACT.Square, accum=s1_[q][:, 2:3])
            for q in range(len(ns)):
                P.act(s1_[q][:, 2:3], s1_[q][:, 2:3], ACT.Sqrt, bias=EPS, scale=1.0 / 256.0)
            for q in range(len(ns)):
                P.recip(s1_[q][:, 3:4], s1_[q][:, 2:3])
            for q, n in enumerate(ns):
                P.stt("dve", y_[q], ob_[q], s1_[q][:, 3:4], sg[j][:, n, :], ALU.mult, ALU.mult)
            pb = psb(bankT).re("p (a t) -> p a t", a=8)
            for q in range(len(ns)):
                for dc in range(2):
                    P.tr(pb[:, 2 * q + dc, :], y_[q][:, dc * 128:(dc + 1) * 128], C.ident)
            for q, n in enumerate(ns):
                P.copy("act", yst[j][:, :, n * 128:(n + 1) * 128], pb[:, 2 * q:2 * q + 2, :])

        stage1a(0)
        for n in range(17):
            if n + 1 < 16:
                stage1a(n + 1)
            if n < 16:
                stage1b(n)
            if n >= 1:
                combine(n - 1)
                if (n - 1) % 4 == 3:
                    norm_batch(list(range(n - 4, n)))
        P.dma("sp", YRv[:, 2 * h:2 * h + 2, :], yst[j], yst[j])
    P.barrier()
    P.release(m)


def phase_nsa(C, QNT, KCT, VCT, KST, VS, KWT, VW, GN, cw, YNT):
    P = C.P
    m = P.mark()
    NH = 16
    SCALE = 128.0 ** -0.5
    BIG = 1.0e5
    BIGS = 30000.0
    slopes = [2.0 ** (-8.0 * (h + 1.0) / NH) for h in range(NH)]
    rotA = Rot(C.banks[0:4])
    obank = C.banks[4:8]

    def iota(v, pattern, base, cm):
        o = v.ap
        P.op("pool", lambda en: en.iota(o, pattern, base=base, channel_multiplier=cm, allow_small_or_imprecise_dtypes=True), writes=[v.buf])

    fregs = {}

    def asel(v, pattern, op, fill, base, cm):
        o = v.ap

        def f(en):
            if fill not in fregs:
                fregs[fill] = en.to_reg(fill)
            return en.affine_select(o, o, pattern, op, fregs[fill], base=base, channel_multiplier=cm)
        P.op("pool", f, reads=[v.buf], writes=[v.buf])

    BIASC = P.sb("BIASC", [128, NH, 127], F32)
    valc = P.sb("valc", [128, 127], F32)
    iota(valc, [[-16, 127]], 1889, 1)
    AL_T = P.sb("AL_T", [128, NH, 128], F32)
    AL_D = P.sb("AL_D", [128, NH, 128], F32)
    AL_W = P.sb("AL_W", [128, NH, 128], F32)
    vala = P.sb("vala", [128, 128], F32)
    iota(vala, [[1, 128]], 0, -1)
    for h in range(NH):
        P.ts("dve", BIASC[:, h, :], valc, -slopes[h], None, ALU.mult)
        asel(BIASC[:, h, :], [[-16, 127]], ALU.is_ge, -BIG, 1889, 1)
        P.ts("dve", AL_T[:, h, :], vala, -slopes[h], None, ALU.mult)
        P.copy("dve", AL_D[:, h, :], AL_T[:, h, :])
        asel(AL_D[:, h, :], [[1, 128]], ALU.is_ge, -BIG, 0, -1)
        P.copy("dve", AL_W[:, h, :], AL_T[:, h, :])
        asel(AL_W[:, h, :], [[-1, 128]], ALU.is_gt, -BIG, 0, 1)
    EALL = P.sb("EALL", [32, T], BF16)
    P.memset("pool", EALL, 1.0)
    asel(EALL, [[1, T]], ALU.is_ge, 0.0, 0, -64)
    asel(EALL, [[-1, T]], ALU.is_ge, 0.0, 63, 64)
    dcur = P.sb("dcur", [128, 64], F32)
    iota(dcur, [[1, 64]], -32, 0)
    pid = P.sb("pid", [128, 1], F32)
    iota(pid, [[0, 1]], 0, 1)
    P.ts("dve", pid, pid, 63.5, None, ALU.is_gt)
    P.ts("dve", dcur, dcur, pid[:, 0:1], None, ALU.subtract)
    KEEP = P.sb("KEEP", [128, 64], F32)
    ADDC = P.sb("ADDC", [128, 64], F32)
    tmpc = P.sb("tmpc", [128, 64], F32)
    P.ts("dve", KEEP, dcur, -1.5, None, ALU.is_lt)
    P.ts("dve", ADDC, dcur, -1.5, 1.0e4, ALU.is_gt, ALU.mult)
    P.ts("dve", tmpc, dcur, 0.5, -10001.0, ALU.is_gt, ALU.mult)
    P.tt("dve", ADDC, ADDC, tmpc, ALU.add)

    kcT = P.sb("kcT", [128, 4, 128], BF16)
    vc = P.sb("vc", [128, 4, 128], BF16)
    P.memset("pool", kcT, 0.0)
    P.memset("pool", vc, 0.0)
    KS = P.sb("KS", [128, 4, T], BF16)
    KW = P.sb("KW", [128, 4, T], BF16)
    VSs = P.sb("VSs", [128, 16, 4, 130], BF16)
    VWs = P.sb("VWs", [128, 16, 4, 130], BF16)
    P.memset("pool", VSs, 1.0)
    P.memset("pool", VWs, 1.0)
    P.dma("sp", KS, KST.re("(g p) t -> p g t", p=128), KS)
    P.dma("sp", KW, KWT.re("(g p) t -> p g t", p=128), KW)
    for c4 in range(16):
        P.dma("sp", VSs[:, c4, :, 0:128], VS.re("(c p) (g d) -> p c g d", p=128, g=4)[:, c4, :, :], VSs)
        P.dma("sp", VWs[:, c4, :, 0:128], VW.re("(c p) (g d) -> p c g d", p=128, g=4)[:, c4, :, :], VWs)

    m1 = P.mark()
    XC = P.sb("XC", [128, 4, T], BF16)
    W1 = P.sb("W1", [128, 32, 128], BF16)
    W2 = P.sb("W2", [128, 128], BF16)
    posr = P.sb("posr", [32, 128], F32)
    posT = P.sb("posT", [128, 32], BF16)
    cb = P.sb("cb", [128, 1], F32)
    hid = P.sb("hid", [128, 4, 127], BF16)
    for which, src in (("k", KCT), ("v", VCT)):
        P.dma("sp", XC, src.re("(g p) t -> p g t", p=128), XC)
        P.dma("pool", W1, cw["w1_" + which].re("(l d) h -> d l h", d=128), W1)
        P.dma("pool", W2, cw["w2_" + which], W2)
        P.dma("sp", posr, cw["pos_" + which], posr)
        b0 = rotA.next()
        P.tr(b0[:, 0:32], posr, C.identf[0:32, 0:32])
        P.copy("dve", posT, b0[:, 0:32])
        b1 = rotA.next()
        for l in range(32):
            P.mm(b1[:, 0:1], W1[:, l, :], posT[:, l:l + 1], start=(l == 0), stop=(l == 31))
        P.copy("dve", cb, b1[:, 0:1])
        b2 = rotA.next()
        b2v = b2[:, 0:508].re("p (g n) -> p g n", g=4)
        for l in range(32):
            P.mm(b2v, W1[:, l, :], XC[:, :, l:l + 2017:16], start=(l == 0), stop=(l == 31))
        P.act(hid, b2v, ACT.Silu, bias=cb[:, 0:1])
        b3 = rotA.next()
        if which == "k":
            b3v = b3[:, 0:508].re("p (g n) -> p g n", g=4)
            P.mm(b3v, W2, hid)
            P.copy("dve", kcT[:, :, 0:127], b3v)
        else:
            for g in range(4):
                P.mm(b3[0:127, g * 128:(g + 1) * 128], hid[:, g, :], W2)
            P.copy("dve", vc[0:127, :, :], b3[0:127, :].re("p (g d) -> p g d", g=4))
    P.barrier()
    P.release(m1)

    qt = [P.sb("qt%d" % i, [128, NH, 128], BF16) for i in range(2)]
    gnt = [P.sb("gnt%d" % i, [128, 48], F32) for i in range(2)]
    osum = [P.sb("osum%d" % i, [128, NH, 128], F32) for i in range(2)]
    ybn = [P.sb("ybn%d" % i, [128, NH * 128], BF16) for i in range(2)]
    ynst = [P.sb("ynst%d" % i, [128, NH, 128], BF16) for i in range(2)]
    s_sb = Rot([P.sb("s_sb%d" % i, [128, 4, 128], F32) for i in range(3)])
    pT = Rot([P.sb("pT%d" % i, [128, 4, 128], BF16) for i in range(3)])
    sc = Rot([P.sb("sc%d" % i, [128, 4, 128], F32) for i in range(2)])
    pe_ = Rot([P.sb("pe%d" % i, [128, 4, 128], F32) for i in range(2)])
    peb = Rot([P.sb("peb%d" % i, [128, 4, 128], BF16) for i in range(2)])
    pTc = Rot([P.sb("pTc%d" % i, [128, 4, 128], BF16) for i in range(2)])
    sm = Rot([P.sb("sm%d" % i, [128, 16], F32) for i in range(4)])
    Pimp = Rot([P.sb("Pimp%d" % i, [128, 128], F32) for i in range(2)])
    imp = Rot([P.sb("imp%d" % i, [128, 32], F32) for i in range(2)])
    cmp3 = P.sb("cmp3", [128, 32, 32], F32)
    cnt = Rot([P.sb("cnt%d" % i, [128, 32], F32) for i in range(2)])
    sbm = Rot([P.sb("sbm%d" % i, [128, 32], BF16) for i in range(2)])
    sbT = Rot([P.sb("sbT%d" % i, [32, 128], BF16) for i in range(2)])
    QNv = QNT.re("(h p) t -> p h t", p=128)
    YNv = YNT.re("(h p) t -> p h t", p=128)

    rotS = Rot(C.banks[0:3])
    rotC = Rot(C.banks[3:4])
    sma = Rot([P.sb("sma%d" % i, [128, 16], F32) for i in range(3)])

    def pull(bg, n):
        if bg is None:
            return
        for _ in range(n):
            try:
                next(bg)
            except StopIteration:
                return

    def chain(i, g, res):
        j = i % 2
        ncm = min(8 * i + 7, 127)
        off = 120 - 8 * i
        ps = rotC.next()
        psv = ps.re("p (r n) -> p r n", r=4)
        for r in range(4):
            P.mm(psv[:, r, 0:ncm], qt[j][:, 4 * g + r, :], kcT[:, g, 0:ncm])
        yield
        s = sc.next()
        P.stt("dve", s[:, :, 0:ncm], psv[:, :, 0:ncm], SCALE, BIASC[:, 4 * g:4 * g + 4, off:off + ncm], ALU.mult, ALU.add)
        yield
        st = sm.next()
        P.red(st[:, 0:4], s[:, :, 0:ncm], ALU.max)
        yield
        P.ts("dve", st[:, 0:4], st[:, 0:4], -1000.0, -1.0, ALU.max, ALU.mult)
        yield
        pp = pe_.next()
        for r in range(4):
            P.act(pp[:, r, 0:ncm], s[:, r, 0:ncm], ACT.Exp, bias=st[:, r:r + 1], accum=st[:, 4 + r:5 + r])
            yield
        P.ts("dve", st[:, 8:12], st[:, 4:8], 1e-30, None, ALU.add)
        yield
        P.recip(st[:, 8:12], st[:, 8:12])
        yield
        pb_ = peb.next()
        P.copy("pool", pb_[:, :, 0:ncm], pp[:, :, 0:ncm])
        yield
        bt = rotC.next()
        btv = psb(bt).re("p (r t) -> p r t", r=8)
        for r in range(4):
            P.tr(btv[0:ncm, r, :], pb_[:, r, 0:ncm], C.ident)
        yield
        ptc = pTc.next()
        P.copy("act", ptc[0:ncm, :, :], btv[0:ncm, 0:4, :])
        yield
        po = rotC.next()
        pov = po.re("p (r d) -> p r d", r=4)
        for r in range(4):
            P.mm(pov[:, r, :], ptc[0:ncm, r, :], vc[0:ncm, g, :])
        yield
        P.tt("dve", st[:, 12:16], st[:, 8:12], mkap(gnt[j][:, 12 * g:12 * g + 1], [[3, 4]]), ALU.mult)
        yield
        for r in range(4):
            P.ts("dve", osum[j][:, 4 * g + r, :], pov[:, r, :], st[:, 12 + r:13 + r], None, ALU.mult)
            yield
        sbt = None
        if i >= 8:
            Pi = Pimp.next()
            P.memset("pool", Pi, 0.0)
            P.ts("dve", Pi[:, 0:ncm], pp[:, 0, 0:ncm], st[:, 8:9], None, ALU.mult)
            yield
            for r in range(1, 4):
                P.stt("dve", Pi[:, 0:ncm], pp[:, r, 0:ncm], st[:, 8 + r:9 + r], Pi[:, 0:ncm], ALU.mult, ALU.add)
                yield
            im = imp.next()
            P.red(im, Pi.re("p (j f) -> p j f", f=4), ALU.add)
            yield
            P3 = mkap(Pi[:, 3:4], [[4, 32]])
            P.stt("dve", im, P3, -0.5, im, ALU.mult, ALU.add)
            yield
            P3s = mkap(Pi[:, 3:4], [[4, 31]])
            P.stt("dve", im[:, 1:32], P3s, 0.5, im[:, 1:32], ALU.mult, ALU.add)
            yield
            lo = 32 - 2 * i
            P.tt("dve", im, im, KEEP[:, lo:lo + 32], ALU.mult)
            yield
            P.tt("dve", im, im, ADDC[:, lo:lo + 32], ALU.add)
            yield
            P.memset("dve", im[:, 0:1], 1.0e4)
            yield
            inA = mkap(im[:, 0:1], [[0, 32], [1, 32]])
            inB = mkap(im[:, 0:1], [[1, 32], [0, 32]])
            P.tt("dve", cmp3, inA, inB, ALU.is_gt)
            yield
            cn = cnt.next()
            P.red(cn, cmp3, ALU.add)
            yield
            sb_ = sbm.next()
            P.ts("dve", sb_, cn, 15.5, -BIGS, ALU.is_ge, ALU.mult)
            yield
            bt2 = rotC.next()
            bt2v = psb(bt2)
            P.tr(bt2v[0:32, 0:128], sb_, C.ident)
            yield
            sbt = sbT.next()
            P.copy("act", sbt, bt2v[0:32, 0:128])
        res["sbt"] = sbt

    def load_tile(i):
        j = i % 2
        cols = slice(i * 128, (i + 1) * 128)
        P.dma("sp", qt[j], QNv[:, :, cols], qt[j])
        P.dma("sp", gnt[j], GN[cols, :], gnt[j])

    order = [(i, g) for i in range(16) for g in range(4)]
    items = []
    for (i, g) in order:
        for mode, Kt, Vt, gidx, chunks in (("sel", KS, VSs, 1, list(range(0, i + 1))),
                                            ("win", KW, VWs, 2, list(range(max(0, i - 4), i + 1)))):
            for ci, c in enumerate(chunks):
                items.append(dict(i=i, g=g, mode=mode, Kt=Kt, Vt=Vt, gidx=gidx, c=c, ci=ci, nch=len(chunks),
                                  first=(mode == "sel" and ci == 0), last_of_tile=(g == 3 and mode == "win" and ci == len(chunks) - 1)))
    chains = {}

    def get_chain(key):
        if key not in chains:
            res = {}
            chains[key] = [chain(key[0], key[1], res), res]
        return chains[key]

    def ensure_chain(key):
        gen, res = get_chain(key)
        for _ in gen:
            pass
        return res

    cur_bg = [None]

    def S1(it):
        i, g = it["i"], it["g"]
        j = i % 2
        if it["first"]:
            if g == 0 and i == 0:
                load_tile(0)
            if g == 1 and i + 1 < 16:
                load_tile(i + 1)
            ensure_chain((i, g))
            k = order.index((i, g))
            cur_bg[0] = get_chain(order[k + 1])[0] if k + 1 < len(order) else None
        sbt = chains[(i, g)][1].get("sbt")
        c = it["c"]
        ps = rotS.next()
        need_mask = (it["mode"] == "sel" and i >= 8 and c < i)
        P.mm(ps, it["Kt"][:, g, c * 128:(c + 1) * 128], qt[j][:, 4 * g:4 * g + 4, :], start=True, stop=not need_mask)
        if need_mask:
            for r in range(4):
                P.mm(ps[:, r * 128:(r + 1) * 128], EALL[:, c * 128:(c + 1) * 128], sbt, start=False, stop=(r == 3))
        it["ps"] = ps

    def S2(it):
        i, g, c = it["i"], it["g"], it["c"]
        d = i - c
        if d == 0:
            AL = AL_D
        elif it["mode"] == "win" and d == 4:
            AL = AL_W
        else:
            AL = AL_T
        s = s_sb.next()
        P.stt("dve", s, it["ps"].re("p (r t) -> p r t", r=4), SCALE, AL[:, 4 * g:4 * g + 4, :], ALU.mult, ALU.add)
        p_ = pT.next()
        for r in range(4):
            P.act(p_[:, r, :], s[:, r, :], ACT.Exp, bias=-slopes[4 * g + r] * 128.0 * d)
        it["p"] = p_

    def S3(it):
        i, g, c, ci, nch = it["i"], it["g"], it["c"], it["ci"], it["nch"]
        j = i % 2
        for r in range(4):
            P.mm(obank[r][:, 0:129], it["p"][:, r, :], it["Vt"][:, c, g, 0:129], start=(ci == 0), stop=(ci == nch - 1))
        pull(cur_bg[0], 4)
        if ci == nch - 1:
            s_ = sma.next()
            for r in range(4):
                h = 4 * g + r
                P.recip(s_[:, r:r + 1], obank[r][:, 128:129])
                P.tt("dve", s_[:, 4 + r:5 + r], s_[:, r:r + 1], gnt[j][:, 3 * h + it["gidx"]:3 * h + it["gidx"] + 1], ALU.mult)
                P.stt("dve", osum[j][:, h, :], obank[r][:, 0:128], s_[:, 4 + r:5 + r], osum[j][:, h, :], ALU.mult, ALU.add)
        if it["last_of_tile"]:
            cols = slice(i * 128, (i + 1) * 128)
            P.copy("pool", ybn[j], osum[j].re("p h d -> p (h d)"))
            for b in range(2):
                bank = rotC.next()
                pb = psb(bank).re("p (a t) -> p a t", a=8)
                for a in range(8):
                    hh = b * 8 + a
                    P.tr(pb[:, a, :], ybn[j][:, hh * 128:(hh + 1) * 128], C.ident)
                P.copy("act", ynst[j][:, b * 8:(b + 1) * 8, :], pb)
            P.dma("sp", YNv[:, :, cols], ynst[j], ynst[j])

    n_it = len(items)
    for k in range(n_it + 2):
        if k < n_it:
            S1(items[k])
        if 0 <= k - 1 < n_it:
            S2(items[k - 1])
        if 0 <= k - 2 < n_it:
            S3(items[k - 2])
    P.barrier()
    P.release(m)


def g1_blocks(S):
    b = []
    b += seg_blocks(0, 2048, "F", "bf16", S["QRT"])
    b += seg_blocks(2048, 2048, "F", "bf16", S["KRT"])
    b += seg_blocks(4096, 2048, "T", "bf16", S["VR"])
    b += seg_blocks(6144, 2048, "T", "silu32", S["GR"])
    b += seg_blocks(8192, 2048, "F", "bf16", S["QNT"])
    b += seg_blocks(10240, 512, "F", "bf16", S["KCT"])
    b += seg_blocks(10752, 512, "F", "bf16", S["VCT"])
    b += seg_blocks(11264, 512, "F", "bf16", S["KST"])
    b += seg_blocks(11776, 512, "T", "bf16", S["VS"])
    b += seg_blocks(12288, 512, "F", "bf16", S["KWT"])
    b += seg_blocks(12800, 512, "T", "bf16", S["VW"])
    b += seg_blocks(13312, 48, "T", "sig32", S["GN"])
    b += seg_blocks(13360, 4096, "F", "sig32", S["GRETT"])
    b += seg_blocks(17456, 4096, "F", "sig32", S["GNSAT"])
    return b


def build_program():
    nc = bass.Bass("TRN2", target_bir_lowering=False)
    C = Ctx(nc)
    x = C.inp("x", [T, D])
    I = {}
    for n, sh in [("mix_norm_pre", [NL, D]), ("w_in", [NL, D, DIN]), ("cmp_pos_k", [NL, 32, 128]), ("cmp_w1_k", [NL, 4096, 128]),
                  ("cmp_w2_k", [NL, 128, 128]), ("cmp_pos_v", [NL, 32, 128]), ("cmp_w1_v", [NL, 4096, 128]), ("cmp_w2_v", [NL, 128, 128]),
                  ("w_ret_up", [NL, 2048, D]), ("w_nsa_up", [NL, 2048, D]), ("w_out", [NL, D, D]), ("mix_norm_post", [NL, D]),
                  ("mlp_norm_pre", [NL, D]), ("w_mlp_in", [NL, D, DFF]), ("w_mlp_out", [NL, DFF, D]), ("mlp_norm_post", [NL, D])]:
        I[n] = C.inp(n, sh)
    ot = nc.dram_tensor("out", [T, D], F32, kind="ExternalOutput")
    out = V(ot.ap(), Buf("out"))
    S = {}
    for n, sh, dt in [("HT", [D, T], BF16), ("QRT", [2048, T], BF16), ("KRT", [2048, T], BF16), ("VR", [T, 2048], BF16), ("GR", [T, 2048], F32), ("QNT", [2048, T], BF16), ("KCT", [512, T], BF16),
                      ("VCT", [512, T], BF16), ("KST", [512, T], BF16), ("VS", [T, 512], BF16), ("KWT", [512, T], BF16),
                      ("VW", [T, 512], BF16), ("GN", [T, 48], F32), ("GRETT", [D, T], F32), ("GNSAT", [D, T], F32),
                      ("YRT", [2048, T], BF16), ("YNT", [2048, T], BF16), ("MT", [D, T], BF16), ("Z", [T, D], F32),
                      ("XA", [T, D], F32), ("XB", [T, D], F32), ("UT", [DFF, T], BF16), ("WB16", [DFF, D], BF16)]:
        S[n] = C.scratch(n, sh, dt)
    phase_nr_a(C, None, x, None, None, I["mix_norm_pre"][0], S["HT"])
    x_cur = x
    for l in range(NL):
        gemm(C, S["HT"], D, I["w_in"][l], g1_blocks(S), T)
        phase_ret(C, S["QRT"], S["KRT"], None, S["VR"], S["GR"], S["YRT"])
        cw = {"pos_k": I["cmp_pos_k"][l], "w1_k": I["cmp_w1_k"][l], "w2_k": I["cmp_w2_k"][l],
              "pos_v": I["cmp_pos_v"][l], "w1_v": I["cmp_w1_v"][l], "w2_v": I["cmp_w2_v"][l]}
        phase_nsa(C, S["QNT"], S["KCT"], S["VCT"], S["KST"], S["VS"], S["KWT"], S["VW"], S["GN"], cw, S["YNT"])
        phase_up(C, S["YRT"], S["YNT"], I["w_ret_up"][l], I["w_nsa_up"][l], S["GRETT"], S["GNSAT"], S["MT"])
        gemm(C, S["MT"], D, I["w_out"][l], seg_blocks(0, D, "T", "f32", S["Z"]), T)
        phase_nr_a(C, S["Z"], x_cur, S["XA"], I["mix_norm_post"][l], I["mlp_norm_pre"][l], S["HT"])
        x_cur = S["XA"]
        wo = I["w_mlp_out"][l]
        bgl = [(S["WB16"][r0:r0 + 512, :], wo[r0:r0 + 512, :]) for r0 in range(0, DFF, 512)]
        gemm(C, S["HT"], D, I["w_mlp_in"][l], seg_blocks(0, DFF, "F", "relu2", S["UT"]), T, bg=bgl)
        gemm(C, S["UT"], DFF, S["WB16"], seg_blocks(0, D, "T", "f32", S["Z"]), 512, KB=16, nwb=3)
        if l < NL - 1:
            phase_nr_a(C, S["Z"], x_cur, S["XB"], I["mlp_norm_post"][l], I["mix_norm_pre"][l + 1], S["HT"])
            x_cur = S["XB"]
        else:
            phase_nr_a(C, S["Z"], x_cur, out, I["mlp_norm_post"][l], None, None)
    C.P.emit()
    return nc


def kernel(**inputs):
    from concourse.bass_utils import run_bass_kernel_spmd
    nc = build_program()
    n = 8
    x = np.ascontiguousarray(np.asarray(inputs["x"], dtype=np.float32))
    shared = {k: np.ascontiguousarray(np.asarray(v, dtype=np.float32)) for k, v in inputs.items() if k != "x"}
    in_maps = []
    for b in range(n):
        mp = dict(shared)
        mp["x"] = x[b]
        in_maps.append(mp)
    res = run_bass_kernel_spmd(nc, in_maps, core_ids=list(range(n)))
    return np.stack([np.asarray(r["out"], dtype=np.float32) for r in res.results], axis=0)
```
